# Optimizing a Trainium2 kernel written in Bass

```python
import jax
import jax.numpy as jnp
from jax import lax
import numpy as np

D_MODEL = 1024
BATCH = 8
SEQ = 4096
DEPTH = 4

GRID_W = 64
CTX_LEN = 256
HEAD_DIM = 64
N_Q_HEADS = D_MODEL // HEAD_DIM
N_KV_HEADS = N_Q_HEADS // 4
GROUP = N_Q_HEADS // N_KV_HEADS
QKV_DIM = (N_Q_HEADS + 2 * N_KV_HEADS) * HEAD_DIM
WINDOW = 128
BLOCK_Q = 128
ROPE_THETA = 10000.0
CONV_WIDTH = 3
N_EXPERTS = 32
TOP_K = 4
D_FF = D_MODEL
SWIGLU_ALPHA = 1.702
SWIGLU_LIMIT = 7.0
EXPERT_BLOCK = 128
N_MIXERS = 3
N_WIN_LAYERS = (DEPTH + 2) // N_MIXERS
N_CONV_LAYERS = (DEPTH + 1) // N_MIXERS
N_FULL_LAYERS = DEPTH // N_MIXERS
DEEPNORM_ALPHA = (2.0 * DEPTH) ** 0.25
DEEPNORM_BETA = (8.0 * DEPTH) ** -0.25
LN_EPS = 1e-5
RMS_EPS = 1e-6
NEG_INF = -1e30

kernel_name = 'hybrid_interleaved_dit_moe'


def layer_norm(x, g, b):
    xf = x.astype(jnp.float32)
    mu = jnp.mean(xf, axis=-1, keepdims=True)
    var = jnp.mean(jnp.square(xf - mu), axis=-1, keepdims=True)
    return ((xf - mu) * lax.rsqrt(var + LN_EPS) * g.astype(jnp.float32) + b.astype(jnp.float32)).astype(x.dtype)


def rms_norm(x, g):
    xf = x.astype(jnp.float32)
    y = xf * lax.rsqrt(jnp.mean(jnp.square(xf), axis=-1, keepdims=True) + RMS_EPS)
    return (y * g.astype(jnp.float32)).astype(x.dtype)


def axial_rope_tables(rows):
    row = jnp.repeat(jnp.arange(rows, dtype=jnp.float32), GRID_W)
    col = jnp.tile(jnp.arange(GRID_W, dtype=jnp.float32), rows)
    n_freq = HEAD_DIM // 4
    inv_freq = ROPE_THETA ** (-jnp.arange(n_freq, dtype=jnp.float32) / n_freq)
    ang = jnp.stack([row[:, None] * inv_freq, col[:, None] * inv_freq], axis=1)
    return jnp.cos(ang), jnp.sin(ang)


def apply_rope(x, cos, sin):
    B, L, H, Dh = x.shape
    xr = x.astype(jnp.float32).reshape(B, L, H, 2, 2, Dh // 4)
    x1, x2 = xr[..., 0, :], xr[..., 1, :]
    cs, sn = cos[None, :, None], sin[None, :, None]
    out = jnp.stack([x1 * cs - x2 * sn, x2 * cs + x1 * sn], axis=-2)
    return out.reshape(B, L, H, Dh).astype(x.dtype)


def project_qkv(h, w, b=None):
    B, L, _ = h.shape
    qkv = h @ w
    if b is not None:
        qkv = qkv + b
    q, k, v = jnp.split(qkv, [N_Q_HEADS * HEAD_DIM, (N_Q_HEADS + N_KV_HEADS) * HEAD_DIM], axis=-1)
    return (q.reshape(B, L, N_Q_HEADS, HEAD_DIM),
            k.reshape(B, L, N_KV_HEADS, HEAD_DIM),
            v.reshape(B, L, N_KV_HEADS, HEAD_DIM))


def attend(q, k, v, mask=None, sink=None):
    s = jnp.einsum('bqhgd,bkhd->bhgqk', q, k).astype(jnp.float32) * (HEAD_DIM ** -0.5)
    if mask is not None:
        s = jnp.where(mask, s, NEG_INF)
    if sink is not None:
        B, H, G, Q, _ = s.shape
        s_sink = jnp.broadcast_to(sink.astype(jnp.float32)[None, :, :, None, None], (B, H, G, Q, 1))
        p = jax.nn.softmax(jnp.concatenate([s, s_sink], axis=-1), axis=-1)[..., :-1]
    else:
        p = jax.nn.softmax(s, axis=-1)
    return jnp.einsum('bhgqk,bkhd->bqhgd', p.astype(v.dtype), v)


def window_attention(hx, hz, w_qkv, b_qkv, sink, w_o, cos, sin, need_ctx):
    B, L, _ = hx.shape
    nb = L // BLOCK_Q
    qx, kx, vx = project_qkv(hx, w_qkv, b_qkv)
    qz, kz, vz = project_qkv(hz, w_qkv, b_qkv)
    qx = apply_rope(qx, cos, sin)
    kx = apply_rope(kx, cos, sin)
    sink_hg = sink.reshape(N_KV_HEADS, GROUP)
    ctx_len = kz.shape[1]

    def band(t):
        tp = jnp.pad(t, ((0, 0), (BLOCK_Q, BLOCK_Q), (0, 0), (0, 0))).reshape(B, nb + 2, BLOCK_Q, N_KV_HEADS, HEAD_DIM)
        return jnp.concatenate([tp[:, :-2], tp[:, 1:-1], tp[:, 2:]], axis=2)

    kb, vb = band(kx), band(vx)
    qb = qx.reshape(B, nb, BLOCK_Q, N_KV_HEADS, GROUP, HEAD_DIM)
    qi = jnp.arange(BLOCK_Q)[:, None]
    kj = jnp.arange(3 * BLOCK_Q)[None, :]
    rel = kj - BLOCK_Q - qi
    ctx_mask = jnp.ones((BLOCK_Q, ctx_len), dtype=bool)

    def block_fn(args):
        q, k, v, blk = args
        key_pos = blk * BLOCK_Q - BLOCK_Q + kj
        m_loc = (jnp.abs(rel) <= WINDOW) & (key_pos >= 0) & (key_pos < L)
        k_cat = jnp.concatenate([k, kz], axis=1)
        v_cat = jnp.concatenate([v, vz], axis=1)
        m_cat = jnp.concatenate([m_loc, ctx_mask], axis=1)
        return attend(q, k_cat, v_cat, mask=m_cat, sink=sink_hg)

    ox = lax.map(block_fn, (jnp.moveaxis(qb, 1, 0), jnp.moveaxis(kb, 1, 0), jnp.moveaxis(vb, 1, 0), jnp.arange(nb)))
    out_x = jnp.moveaxis(ox, 0, 1).reshape(B, L, D_MODEL) @ w_o
    out_z = None
    if need_ctx:
        oz = attend(qz.reshape(B, ctx_len, N_KV_HEADS, GROUP, HEAD_DIM), kz, vz, sink=sink_hg)
        out_z = oz.reshape(B, ctx_len, D_MODEL) @ w_o
    return out_x, out_z


def full_attention(hx, hz, w_qkv, q_norm, k_norm, w_o, cos, sin, need_ctx):
    B, L, _ = hx.shape
    nb = L // BLOCK_Q
    qx, kx, vx = project_qkv(hx, w_qkv)
    qz, kz, vz = project_qkv(hz, w_qkv)
    qx = apply_rope(rms_norm(qx, q_norm), cos, sin)
    kx = apply_rope(rms_norm(kx, k_norm), cos, sin)
    qz = rms_norm(qz, q_norm)
    kz = rms_norm(kz, k_norm)
    ctx_len = kz.shape[1]
    k_all = jnp.concatenate([kx, kz], axis=1)
    v_all = jnp.concatenate([vx, vz], axis=1)
    qb = jnp.moveaxis(qx.reshape(B, nb, BLOCK_Q, N_KV_HEADS, GROUP, HEAD_DIM), 1, 0)
    ox = lax.map(lambda q: attend(q, k_all, v_all), qb)
    out_x = jnp.moveaxis(ox, 0, 1).reshape(B, L, D_MODEL) @ w_o
    out_z = None
    if need_ctx:
        oz = attend(qz.reshape(B, ctx_len, N_KV_HEADS, GROUP, HEAD_DIM), kz, vz)
        out_z = oz.reshape(B, ctx_len, D_MODEL) @ w_o
    return out_x, out_z


def depthwise_conv_centred(u, w):
    pad = CONV_WIDTH // 2
    return lax.conv_general_dilated(u, w[:, None, :], window_strides=(1,), padding=((pad, pad),),
                                    dimension_numbers=('NWC', 'WIO', 'NWC'), feature_group_count=u.shape[-1])


def short_conv_mixer(h, w_in, w_conv, w_out):
    b_gate, c_gate, xv = jnp.split(h @ w_in, 3, axis=-1)
    return (b_gate * depthwise_conv_centred(c_gate * xv, w_conv)) @ w_out


def moe_ffn(h, router_w, router_b, w_gu, b_gu, w_down, b_down):
    T, D = h.shape
    logits = (h @ router_w + router_b).astype(jnp.float32)
    top_val, top_idx = lax.top_k(logits, TOP_K)
    gates = jax.nn.softmax(top_val, axis=-1)
    A = T * TOP_K
    e_flat = top_idx.reshape(A)
    tok_flat = jnp.broadcast_to(jnp.arange(T, dtype=jnp.int32)[:, None], (T, TOP_K)).reshape(A)
    g_flat = gates.reshape(A)
    order = jnp.argsort(e_flat)
    e_sorted = e_flat[order]
    counts = jnp.bincount(e_flat, length=N_EXPERTS)
    starts = jnp.cumsum(counts) - counts
    padded = (counts + EXPERT_BLOCK - 1) // EXPERT_BLOCK * EXPERT_BLOCK
    pends = jnp.cumsum(padded)
    pstarts = pends - padded
    dest = pstarts[e_sorted] + (jnp.arange(A) - starts[e_sorted])
    nblk = -(-A // EXPERT_BLOCK) + N_EXPERTS
    P = nblk * EXPERT_BLOCK
    tok_buf = jnp.full((P,), T, dtype=jnp.int32).at[dest].set(tok_flat[order])
    g_buf = jnp.zeros((P,), dtype=h.dtype).at[dest].set(g_flat[order].astype(h.dtype))
    blk_expert = jnp.minimum(jnp.searchsorted(pends, jnp.arange(nblk) * EXPERT_BLOCK, side='right'), N_EXPERTS - 1)
    h_pad = jnp.concatenate([h, jnp.zeros((1, D), h.dtype)], axis=0)

    def expert_block(args):
        toks, e = args
        xb = h_pad[toks]
        gate, up = jnp.split(xb @ w_gu[e] + b_gu[e], 2, axis=-1)
        gate = jnp.minimum(gate, SWIGLU_LIMIT)
        up = jnp.clip(up, -SWIGLU_LIMIT, SWIGLU_LIMIT)
        act = (up + 1) * gate * jax.nn.sigmoid(SWIGLU_ALPHA * gate)
        return act @ w_down[e] + b_down[e]

    out = lax.map(expert_block, (tok_buf.reshape(nblk, EXPERT_BLOCK), blk_expert)).reshape(P, D)
    y = jnp.zeros((T + 1, D), dtype=h.dtype).at[tok_buf].add(out * g_buf[:, None])
    return y[:T]


def setup_inputs(seed: int = 0) -> dict:
    key = jax.random.key(seed)
    ks = iter(jax.random.split(key, 32))
    D = D_MODEL
    s = D ** -0.5

    def nrm(shape, std):
        return std * jax.random.normal(next(ks), shape, jnp.float32)

    return {
        'x': nrm((BATCH, SEQ, D), 1.0),
        'c': nrm((BATCH, D), 1.0),
        'ctx': nrm((BATCH, CTX_LEN, D), 1.0),
        'c_ctx': nrm((D,), 1.0),
        'mod_w': nrm((DEPTH, D, 6 * D), s),
        'mod_b': nrm((DEPTH, 6 * D), 0.01),
        'ln1_g': 1.0 + nrm((DEPTH, D), 0.01),
        'ln1_b': nrm((DEPTH, D), 0.01),
        'ln2_g': 1.0 + nrm((DEPTH, D), 0.01),
        'ln2_b': nrm((DEPTH, D), 0.01),
        'win_wqkv': nrm((N_WIN_LAYERS, D, QKV_DIM), s),
        'win_bqkv': nrm((N_WIN_LAYERS, QKV_DIM), 0.01),
        'win_sink': nrm((N_WIN_LAYERS, N_Q_HEADS), 0.5),
        'win_wo': nrm((N_WIN_LAYERS, D, D), s * DEEPNORM_BETA),
        'conv_win': nrm((N_CONV_LAYERS, D, 3 * D), s),
        'conv_w': nrm((N_CONV_LAYERS, CONV_WIDTH, D), CONV_WIDTH ** -0.5),
        'conv_wout': nrm((N_CONV_LAYERS, D, D), s * DEEPNORM_BETA),
        'full_wqkv': nrm((N_FULL_LAYERS, D, QKV_DIM), s),
        'full_qnorm': 1.0 + nrm((N_FULL_LAYERS, HEAD_DIM), 0.01),
        'full_knorm': 1.0 + nrm((N_FULL_LAYERS, HEAD_DIM), 0.01),
        'full_wo': nrm((N_FULL_LAYERS, D, D), s * DEEPNORM_BETA),
        'router_w': nrm((DEPTH, D, N_EXPERTS), s),
        'router_b': nrm((DEPTH, N_EXPERTS), 0.01),
        'expert_wgu': nrm((DEPTH, N_EXPERTS, D, 2 * D_FF), s),
        'expert_bgu': nrm((DEPTH, N_EXPERTS, 2 * D_FF), 0.01),
        'expert_wdown': nrm((DEPTH, N_EXPERTS, D_FF, D), D_FF ** -0.5 * DEEPNORM_BETA),
        'expert_bdown': nrm((DEPTH, N_EXPERTS, D), 0.01),
    }


def reference(x, c, ctx, c_ctx, mod_w, mod_b, ln1_g, ln1_b, ln2_g, ln2_b,
              win_wqkv, win_bqkv, win_sink, win_wo, conv_win, conv_w, conv_wout,
              full_wqkv, full_qnorm, full_knorm, full_wo,
              router_w, router_b, expert_wgu, expert_bgu, expert_wdown, expert_bdown):
    B, L, D = x.shape
    ROWS = L // GRID_W
    cos, sin = axial_rope_tables(ROWS)
    silu_c = jax.nn.silu(c)
    silu_cz = jax.nn.silu(c_ctx)
    z = ctx
    for i in range(DEPTH):
        kind, j = i % N_MIXERS, i // N_MIXERS
        need_ctx = i < DEPTH - 1
        mx = (silu_c @ mod_w[i] + mod_b[i])[:, None, :]
        sh1, sc1, g1, sh2, sc2, g2 = jnp.split(mx, 6, axis=-1)
        mz = silu_cz @ mod_w[i] + mod_b[i]
        zsh1, zsc1, zg1, zsh2, zsc2, zg2 = jnp.split(mz, 6, axis=-1)

        hx = x * (1 + sc1) + sh1
        hz = z * (1 + zsc1) + zsh1
        if kind == 0:
            ox, oz = window_attention(hx, hz, win_wqkv[j], win_bqkv[j], win_sink[j], win_wo[j], cos, sin, need_ctx)
        elif kind == 1:
            ox = short_conv_mixer(hx, conv_win[j], conv_w[j], conv_wout[j])
            oz = short_conv_mixer(hz, conv_win[j], conv_w[j], conv_wout[j]) if need_ctx else None
        else:
            ox, oz = full_attention(hx, hz, full_wqkv[j], full_qnorm[j], full_knorm[j], full_wo[j], cos, sin, need_ctx)
        x = layer_norm(DEEPNORM_ALPHA * x + g1 * ox, ln1_g[i], ln1_b[i])

        hx = x * (1 + sc2) + sh2
        moe_args = (router_w[i], router_b[i], expert_wgu[i], expert_bgu[i], expert_wdown[i], expert_bdown[i])
        if need_ctx:
            z = layer_norm(DEEPNORM_ALPHA * z + zg1 * oz, ln1_g[i], ln1_b[i])
            hz = z * (1 + zsc2) + zsh2
            ctx_len = z.shape[1]
            tokens = jnp.concatenate([hx.reshape(B * L, D), hz.reshape(B * ctx_len, D)], axis=0)
            f = moe_ffn(tokens, *moe_args)
            fx = f[:B * L].reshape(B, L, D)
            fz = f[B * L:].reshape(B, ctx_len, D)
            z = layer_norm(DEEPNORM_ALPHA * z + zg2 * fz, ln2_g[i], ln2_b[i])
        else:
            fx = moe_ffn(hx.reshape(B * L, D), *moe_args).reshape(B, L, D)
        x = layer_norm(DEEPNORM_ALPHA * x + g2 * fx, ln2_g[i], ln2_b[i])
    return x
```

```python
from contextlib import ExitStack, contextmanager
import numpy as np
import concourse.bass as bass
import concourse.mybir as mybir
from concourse.bass_utils import run_bass_kernel_spmd

F32 = mybir.dt.float32
BF16 = mybir.dt.bfloat16
I32 = mybir.dt.int32
AF = mybir.ActivationFunctionType
ALU = mybir.AluOpType
AX = mybir.AxisListType


class Buf:
    __slots__ = ("name", "h", "last_w", "readers", "epoch")

    def __init__(self, name, h=None):
        self.name = name
        self.h = h
        self.last_w = None
        self.readers = {}
        self.epoch = 0

    def ap(self):
        return self.h[:]

    def __getitem__(self, idx):
        return self.h[idx]


class K:
    NSLOT = 4

    def __init__(self, nc):
        self.nc = nc
        self.stack = ExitStack()
        self.eng = {"pe": nc.tensor, "act": nc.scalar, "dve": nc.vector, "pool": nc.gpsimd, "sp": nc.sync}
        self.NSETS = 5
        self.semsets = [{} for _ in range(self.NSETS)]
        self.count = {}
        self.seen = {e: {} for e in self.eng}
        self.slots = {}
        self.slot_rr = {}
        for e in self.eng:
            for si in range(self.NSETS):
                self.semsets[si][e] = self.stack.enter_context(nc.semaphore(f"s{si}_{e}"))
            self.count[e] = 0
        for q in ("sp", "act", "pool"):
            self.slots[q] = []
            for i in range(self.NSLOT):
                key = f"d_{q}{i}"
                for si in range(self.NSETS):
                    self.semsets[si][key] = self.stack.enter_context(nc.semaphore(f"s{si}_{key}"))
                self.count[key] = 0
                self.slots[q].append(key)
            self.slot_rr[q] = 0
        self.sems = self.semsets[0]
        self.out_buf = Buf("out")
        self.scopes = [self.stack]
        self.n_inst = 0
        self.epoch = 0
        self.round = 0

    def _uniq(self, name):
        self._nuniq = getattr(self, "_nuniq", 0) + 1
        return f"{name}_{self._nuniq}"

    def sb(self, name, shape, dtype):
        h = self.scopes[-1].enter_context(self.nc.sbuf_tensor(self._uniq("sb_" + name), list(shape), dtype))
        return Buf(name, h)

    def ps(self, name, shape, dtype):
        h = self.scopes[-1].enter_context(self.nc.psum_tensor(self._uniq("ps_" + name), list(shape), dtype))
        return Buf(name, h)

    @contextmanager
    def scope(self):
        st = ExitStack()
        self.scopes.append(st)
        try:
            yield
        finally:
            self.barrier()
            self.scopes.pop()
            st.close()

    def _wait(self, e, key, val):
        if val <= 0:
            return
        assert val < 60000, (key, val)
        if self.seen[e].get(key, 0) >= val:
            return
        self.eng[e].wait_ge(self.sems[key], val)
        self.seen[e][key] = val
        self.n_inst += 1

    def _deps(self, e, reads, writes):
        need = {}
        for b in list(reads) + list(writes):
            if b.epoch != self.epoch:
                b.epoch = self.epoch
                b.last_w = None
                b.readers = {}
        for b in reads:
            if b.last_w is not None:
                k_, v = b.last_w
                need[k_] = max(need.get(k_, 0), v)
        for b in writes:
            if b.last_w is not None:
                k_, v = b.last_w
                need[k_] = max(need.get(k_, 0), v)
            for k_, v in b.readers.items():
                need[k_] = max(need.get(k_, 0), v)
        return need

    def _record(self, ev, reads, writes):
        k_, v = ev
        for b in reads:
            if b.readers.get(k_, 0) < v:
                b.readers[k_] = v
        for b in writes:
            b.last_w = ev
            b.readers = {}

    def op(self, e, fn, reads=(), writes=(), inc=True):
        need = self._deps(e, reads, writes)
        for k_, v in need.items():
            if e == "pe" and k_ == "pe":
                continue
            self._wait(e, k_, v)
        ins = fn(self.eng[e])
        self.n_inst += 1
        if inc:
            ins.then_inc(self.sems[e], 1)
            self.count[e] += 1
            ev = (e, self.count[e])
        else:
            ev = (e, self.count[e] + 1)
        self._record(ev, reads, writes)
        return ev

    def dma(self, q, out, in_, reads=(), writes=(), **kw):
        need = self._deps(q, reads, writes)
        for k_, v in need.items():
            self._wait(q, k_, v)
        i = self.slot_rr[q]
        self.slot_rr[q] = (i + 1) % self.NSLOT
        key = self.slots[q][i]
        self._wait(q, key, self.count[key])
        ins = self.eng[q].dma_start(out=out, in_=in_, **kw)
        ins.then_inc(self.sems[key], 16)
        self.n_inst += 1
        self.count[key] += 16
        ev = (key, self.count[key])
        self._record(ev, reads, writes)
        return ev

    def barrier(self, reset=True):
        import os
        if os.environ.get("NORESET"):
            reset = False
        for e in self.eng:
            for key in self.sems:
                if key == e:
                    continue
                self._wait(e, key, self.count[key])
        if not reset:
            return
        if max(self.count.values()) < 12000:
            return
        self.round += 1
        assert self.round < self.NSETS
        self.sems = self.semsets[self.round]
        for key in self.count:
            self.count[key] = 0
        self.seen = {e: {} for e in self.eng}
        self.epoch += 1

    def finish(self):
        self.barrier(reset=False)
        self.stack.close()


D = 1024
L = 4096
CL = 256
NTL = 32
NT = 34
DEPTH = 4
NE = 32
ALPHA = (2.0 * DEPTH) ** 0.25
LN_EPS = 1e-5
RMS_EPS = 1e-6


class Rot:
    def __init__(self, bufs):
        self.bufs = list(bufs)
        self.i = 0

    def next(self):
        b = self.bufs[self.i % len(self.bufs)]
        self.i += 1
        return b


class Prog:
    def __init__(self, n_layers=DEPTH, first_layer=0, debug=False, final=True):
        self.n_layers = n_layers
        self.first_layer = first_layer
        self.final = final
        nc = bass.Bass("TRN2", target_bir_lowering=False)
        self.nc = nc
        self.k = K(nc)

        def din(name, shape, dt=F32):
            return nc.dram_tensor(name, list(shape), dt, kind="ExternalInput").ap()

        layers = list(range(first_layer, first_layer + n_layers))
        nl = n_layers
        self.jw = {j: n for n, j in enumerate(sorted({i // 3 for i in layers if i % 3 == 0}))}
        self.jc = {j: n for n, j in enumerate(sorted({i // 3 for i in layers if i % 3 == 1}))}
        self.jf = {j: n for n, j in enumerate(sorted({i // 3 for i in layers if i % 3 == 2}))}
        nw, ncv, nf = max(1, len(self.jw)), max(1, len(self.jc)), max(1, len(self.jf))
        self.x = din("x", [L, D])
        self.ctx = din("ctx", [CL, D])
        self.ccT = din("ccT", [128, 16])
        self.mod_w = din("mod_w", [nl, D, 6 * D])
        self.mod_b = din("mod_b", [nl, 6 * D])
        self.ln1_g = din("ln1_g", [nl, D])
        self.ln1_b = din("ln1_b", [nl, D])
        self.ln2_g = din("ln2_g", [nl, D])
        self.ln2_b = din("ln2_b", [nl, D])
        self.win_wqkv = din("win_wqkv", [nw, D, 1536])
        self.win_bqkv = din("win_bqkv", [nw, 1536])
        self.win_sink = din("win_sink", [nw, 16])
        self.win_wo = din("win_wo", [nw, D, D])
        self.conv_win = din("conv_win", [ncv, D, 3 * D])
        self.conv_wT = din("conv_wT", [ncv, 128, 8, 3])
        self.conv_wout = din("conv_wout", [ncv, D, D])
        self.full_wqkv = din("full_wqkv", [nf, D, 1536])
        self.full_qnorm = din("full_qnorm", [nf, 64])
        self.full_knorm = din("full_knorm", [nf, 64])
        self.full_wo = din("full_wo", [nf, D, D])
        self.router_w = din("router_w", [nl, D, NE])
        self.router_b = din("router_b", [nl, NE])
        self.expert_wgu = din("expert_wgu", [nl, NE, D, 2 * D])
        self.expert_bguT = din("expert_bguT", [nl, 128, NE, 16])
        self.expert_wdown = din("expert_wdown", [nl, NE, D, D])
        self.expert_bdown = din("expert_bdown", [nl, NE, D])
        self.c_ident = din("c_ident", [128, 128])
        self.c_cos = din("c_cos", [128, NTL, 32])
        self.c_sin = din("c_sin", [128, NTL, 32])
        self.c_mask = din("c_mask", [128, 2, 128])
        if final:
            self.out = nc.dram_tensor("out", [L, D], F32, kind="ExternalOutput").ap()
        else:
            self.out = nc.dram_tensor("xs", [NT * 128, D], F32, kind="ExternalOutput").ap()
        self.X = nc.dram_tensor("Xs", [NT * 128, D], F32, kind="Internal").ap()
        self.MODV = nc.dram_tensor("MODV", [4, 2, 6 * D], F32, kind="Internal").ap()
        self.GTD = nc.dram_tensor("GTD", [NE, NT * 128], F32, kind="Internal").ap()
        self.MT = nc.dram_tensor("MTs", [8, 128, NT * 128], BF16, kind="Internal").ap()
        self.Xd = [Buf(f"X{t}") for t in range(NT)]
        self.MODVd = Buf("MODVd")
        self.GTDd = Buf("GTDd")
        self.MTd = Buf("MTd")
        self.debug = debug

    def consts(self):
        k = self.k
        self.identf = k.sb("identf", [128, 128], F32)
        k.dma("sp", self.identf.ap(), self.c_ident, [], [self.identf])
        self.identb = k.sb("identb", [128, 128], BF16)
        k.dma("pool", self.identb.ap(), self.c_ident, [], [self.identb])
        self.eps_ln = k.sb("eps_ln", [128, 1], F32)
        k.op("dve", lambda e: e.memset(self.eps_ln.ap(), LN_EPS), [], [self.eps_ln])
        self.eps_rms = k.sb("eps_rms", [128, 1], F32)
        k.op("dve", lambda e: e.memset(self.eps_rms.ap(), RMS_EPS), [], [self.eps_rms])
        self.onesf = k.sb("onesf", [128, 64], F32)
        k.op("dve", lambda e: e.memset(self.onesf.ap(), 1.0), [], [self.onesf])

    def load_bc(self, buf, src_row, srcbuf=None, q="sp"):
        n = buf.ap().shape[0]
        self.k.dma(q, buf.ap(), src_row.partition_broadcast(n), [srcbuf] if srcbuf else [], [buf])

    def prologue(self):
        k = self.k
        for t in range(NT):
            src = self.x[t * 128:(t + 1) * 128, :] if t < NTL else self.ctx[(t - NTL) * 128:(t - NTL + 1) * 128, :]
            k.dma("sp" if t % 2 else "act", self.X[t * 128:(t + 1) * 128, :], src, [], [self.Xd[t]])
        with k.scope():
            cc = k.sb("cc", [128, 16], F32)
            k.dma("sp", cc.ap(), self.ccT, [], [cc])
            sT = k.sb("sT", [128, 16], F32)
            k.op("act", lambda e: e.activation(out=sT.ap(), in_=cc.ap(), func=AF.Silu), [cc], [sT])
            wb = Rot([k.sb(f"mw{i}", [128, 8, 512], F32) for i in range(2)])
            pp = Rot([k.ps(f"mps{i}", [2, 512], F32) for i in range(2)])
            mv = k.sb("mv", [2, 6 * D], F32)
            mb = k.sb("mb", [2, 6 * D], F32)
            for i in range(self.first_layer, self.first_layer + self.n_layers):
                self.load_bc(mb, self.mod_b[i - self.first_layer], q="act")
                for n in range(12):
                    w = wb.next()
                    p = pp.next()
                    k.dma("sp", w.ap(), self.mod_w[i - self.first_layer][:, n * 512:(n + 1) * 512].rearrange("(kc p) n -> p kc n", p=128), [], [w])
                    for kc in range(8):
                        k.op("pe", lambda e: e.matmul(p.ap(), sT.ap()[:, 2 * kc:2 * kc + 2], w.ap()[:, kc, :],
                                                      start=(kc == 0), stop=(kc == 7)), [sT, w], [p], inc=(kc == 7))
                    k.op("dve", lambda e: e.tensor_tensor(out=mv.ap()[:, n * 512:(n + 1) * 512], in0=p.ap(),
                                                          in1=mb.ap()[:, n * 512:(n + 1) * 512], op=ALU.add), [p, mb], [mv])
                for seg in (1, 4):
                    sl = mv.ap()[:, seg * D:(seg + 1) * D]
                    k.op("dve", lambda e: e.tensor_scalar(out=sl, in0=sl, scalar1=1.0, scalar2=None, op0=ALU.add), [mv], [mv])
                k.dma("sp", self.MODV[i], mv.ap(), [mv], [self.MODVd])

    def load_mod(self, i, ty, seg, buf):
        self.load_bc(buf, self.MODV[i, ty, seg * D:(seg + 1) * D], self.MODVd)

    def resid_ln(self, yp, xt, gbc, lng, lnb, u, xo, st6, mvv, rstd):
        k = self.k
        for n in range(2):
            ap_, b_ = yp[n]
            k.op("dve", lambda e: e.tensor_tensor(out=u.ap()[:, n * 512:(n + 1) * 512], in0=ap_,
                                                  in1=gbc.ap()[:, n * 512:(n + 1) * 512], op=ALU.mult), [b_, gbc], [u])
        k.op("dve", lambda e: e.scalar_tensor_tensor(out=u.ap(), in0=xt.ap(), scalar=ALPHA, in1=u.ap(),
                                                     op0=ALU.mult, op1=ALU.add), [xt, u], [u])
        for n in range(2):
            k.op("dve", lambda e: e.bn_stats(out=st6.ap()[:, n * 6:(n + 1) * 6], in_=u.ap()[:, n * 512:(n + 1) * 512]), [u], [st6])
        k.op("dve", lambda e: e.bn_aggr(out=mvv.ap(), in_=st6.ap()), [st6], [mvv])
        k.op("act", lambda e: e.activation(out=rstd.ap(), in_=mvv.ap()[:, 1:2], func=AF.Sqrt, bias=self.eps_ln.ap(), scale=1.0),
             [mvv, self.eps_ln], [rstd])
        k.op("dve", lambda e: e.reciprocal(out=rstd.ap(), in_=rstd.ap()), [rstd], [rstd])
        k.op("dve", lambda e: e.tensor_scalar(out=u.ap(), in0=u.ap(), scalar1=mvv.ap()[:, 0:1], scalar2=rstd.ap(),
                                              op0=ALU.subtract, op1=ALU.mult), [u, mvv, rstd], [u])
        k.op("pool", lambda e: e.tensor_tensor(out=u.ap(), in0=u.ap(), in1=lng.ap(), op=ALU.mult), [u, lng], [u])
        k.op("pool", lambda e: e.tensor_tensor(out=xo.ap(), in0=u.ap(), in1=lnb.ap(), op=ALU.add), [u, lnb], [xo])

    def attention_layer(self, i, full, j, need_ctx, last):
        k = self.k
        nq = NT if need_ctx else NTL
        j = self.jf[j] if full else self.jw[j]
        wqkv_d = self.full_wqkv[j] if full else self.win_wqkv[j]
        wo_d = self.full_wo[j] if full else self.win_wo[j]
        with k.scope():
            KT = k.sb("KT", [64, 4, NT * 128], BF16)
            V = k.sb("V", [128, NT, 4, 65], BF16)
            k.op("pool", lambda e: e.memset(V.ap(), 1.0), [], [V])
            COS = k.sb("COS", [128, NTL, 32], F32)
            SIN = k.sb("SIN", [128, NTL, 32], F32)
            k.dma("sp", COS.ap(), self.c_cos, [], [COS])
            k.dma("act", SIN.ap(), self.c_sin, [], [SIN])
            modA = k.sb("modA", [128, D], F32)
            modB = k.sb("modB", [128, D], F32)
            PT0 = k.ps("PT0", [128, 1024], BF16)
            PT1 = k.ps("PT1", [128, 1024], BF16)
            PA = k.ps("PA", [128, 512], F32)
            PB = k.ps("PB", [128, 512], F32)
            xts = Rot([k.sb(f"xt{n}", [128, D], F32) for n in range(2)])
            tmp1 = k.sb("tmp1", [128, D], F32)
            tmp2 = k.sb("tmp2", [128, D], F32)
            hbs = Rot([k.sb(f"hb{n}", [128, D], BF16) for n in range(2)])
            hTs = Rot([k.sb(f"hT{n}", [128, D], BF16) for n in range(2)])
            qs = k.sb("qs", [128, D], F32)
            qb = k.sb("qb", [128, D], BF16)
            ms = k.sb("ms", [128, 16], F32)
            if full:
                qn = k.sb("qn", [128, 64], F32)
                kn = k.sb("kn", [128, 64], F32)
                self.load_bc(qn, self.full_qnorm[j])
                self.load_bc(kn, self.full_knorm[j])
            else:
                bias = k.sb("bias", [128, 1536], F32)
                self.load_bc(bias, self.win_bqkv[j])
            cur_ty = [-1]

            def front(t):
                ty = 0 if t < NTL else 1
                if cur_ty[0] != ty:
                    self.load_mod(i, ty, 1, modA)
                    self.load_mod(i, ty, 0, modB)
                    cur_ty[0] = ty
                xt = xts.next()
                k.dma("sp", xt.ap(), self.X[t * 128:(t + 1) * 128, :], [self.Xd[t]], [xt])
                k.op("pool", lambda e: e.tensor_tensor(out=tmp1.ap(), in0=xt.ap(), in1=modA.ap(), op=ALU.mult), [xt, modA], [tmp1])
                hb = hbs.next()
                k.op("dve", lambda e: e.tensor_tensor(out=hb.ap(), in0=tmp1.ap(), in1=modB.ap(), op=ALU.add), [tmp1, modB], [hb])
                for c in range(8):
                    k.op("pe", lambda e: e.transpose(PT0.ap()[:, c * 128:(c + 1) * 128], hb.ap()[:, c * 128:(c + 1) * 128], self.identb.ap()),
                         [hb, self.identb], [PT0], inc=(c == 7))
                hT = hTs.next()
                k.op("act", lambda e: e.copy(out=hT.ap(), in_=PT0.ap()), [PT0], [hT])
                return xt, hT

            def process_qk(src, H, t, gn, dst):
                W = H * 64
                s2 = src.ap()[:, 0:W]
                s3 = s2.rearrange("p (h d) -> p h d", d=64)
                if full:
                    k.op("dve", lambda e: e.tensor_tensor(out=tmp1.ap()[:, 0:W], in0=s2, in1=s2, op=ALU.mult), [src], [tmp1])
                    k.op("dve", lambda e: e.tensor_reduce(out=ms.ap()[:, 0:H], in_=tmp1.ap()[:, 0:W].rearrange("p (h d) -> p h d", d=64),
                                                          axis=AX.X, op=ALU.add), [tmp1], [ms])
                    k.op("act", lambda e: e.activation(out=ms.ap()[:, 0:H], in_=ms.ap()[:, 0:H], func=AF.Sqrt,
                                                       bias=self.eps_rms.ap(), scale=1.0 / 64.0), [ms, self.eps_rms], [ms])
                    k.op("dve", lambda e: e.reciprocal(out=ms.ap()[:, 0:H], in_=ms.ap()[:, 0:H]), [ms], [ms])
                    k.op("dve", lambda e: e.tensor_tensor(out=s3, in0=s3, in1=ms.ap()[:, 0:H].unsqueeze(2).to_broadcast([128, H, 64]),
                                                          op=ALU.mult), [src, ms], [src])
                    k.op("pool", lambda e: e.tensor_tensor(out=s3, in0=s3, in1=gn.ap().unsqueeze(1).to_broadcast([128, H, 64]),
                                                           op=ALU.mult), [src, gn], [src])
                d2 = dst.ap()[:, 0:W]
                if t < NTL:
                    v5 = s2.rearrange("p (h a b f) -> p h a b f", a=2, b=2, f=16)
                    x1 = v5[:, :, :, 0, :]
                    x2 = v5[:, :, :, 1, :]
                    o5 = d2.rearrange("p (h a b f) -> p h a b f", a=2, b=2, f=16)
                    cs = COS.ap()[:, t, :].rearrange("p (a f) -> p a f", a=2).unsqueeze(1).to_broadcast([128, H, 2, 16])
                    sn = SIN.ap()[:, t, :].rearrange("p (a f) -> p a f", a=2).unsqueeze(1).to_broadcast([128, H, 2, 16])
                    WH = W // 2
                    ta = tmp1.ap()[:, 0:WH].rearrange("p (h a f) -> p h a f", a=2, f=16)
                    tb = tmp2.ap()[:, 0:WH].rearrange("p (h a f) -> p h a f", a=2, f=16)
                    tc_ = tmp1.ap()[:, WH:W].rearrange("p (h a f) -> p h a f", a=2, f=16)
                    td = tmp2.ap()[:, WH:W].rearrange("p (h a f) -> p h a f", a=2, f=16)
                    k.op("dve", lambda e: e.tensor_tensor(out=ta, in0=x1, in1=cs, op=ALU.mult), [src, COS], [tmp1])
                    k.op("pool", lambda e: e.tensor_tensor(out=tb, in0=x2, in1=sn, op=ALU.mult), [src, SIN], [tmp2])
                    k.op("dve", lambda e: e.tensor_tensor(out=tc_, in0=x2, in1=cs, op=ALU.mult), [src, COS], [tmp1])
                    k.op("pool", lambda e: e.tensor_tensor(out=td, in0=x1, in1=sn, op=ALU.mult), [src, SIN], [tmp2])
                    k.op("dve", lambda e: e.tensor_tensor(out=o5[:, :, :, 0, :], in0=ta, in1=tb, op=ALU.subtract), [tmp1, tmp2], [dst])
                    k.op("dve", lambda e: e.tensor_tensor(out=o5[:, :, :, 1, :], in0=tc_, in1=td, op=ALU.add), [tmp1, tmp2], [dst])
                else:
                    k.op("dve", lambda e: e.tensor_copy(out=d2, in_=s2), [src], [dst])

            with k.scope():
                Wkv = k.sb("Wkv", [128, 8, 512], BF16)
                for kc in range(8):
                    k.dma("pool", Wkv.ap()[:, kc, :], wqkv_d[kc * 128:(kc + 1) * 128, 1024:1536], [], [Wkv])
                for t in range(NT):
                    xt, hT = front(t)
                    for kc in range(8):
                        k.op("pe", lambda e: e.matmul(PA.ap(), hT.ap()[:, kc * 128:(kc + 1) * 128], Wkv.ap()[:, kc, :],
                                                      start=(kc == 0), stop=(kc == 7)), [hT, Wkv], [PA], inc=(kc == 7))
                    if full:
                        k.op("act", lambda e: e.copy(out=qs.ap()[:, 0:512], in_=PA.ap()), [PA], [qs])
                    else:
                        k.op("dve", lambda e: e.tensor_tensor(out=qs.ap()[:, 0:512], in0=PA.ap(), in1=bias.ap()[:, 1024:1536], op=ALU.add),
                             [PA, bias], [qs])
                    k.op("act", lambda e: e.copy(out=V.ap()[:, t, :, 0:64], in_=qs.ap()[:, 256:512].rearrange("p (g d) -> p g d", d=64)),
                         [qs], [V])
                    process_qk(qs, 4, t, kn if full else None, qb)
                    for g in range(4):
                        k.op("pe", lambda e: e.transpose(PT1.ap()[0:64, g * 128:(g + 1) * 128], qb.ap()[:, g * 64:(g + 1) * 64], self.identb.ap()),
                             [qb, self.identb], [PT1], inc=(g == 3))
                    k.op("act", lambda e: e.copy(out=KT.ap()[:, :, t * 128:(t + 1) * 128],
                                                 in_=PT1.ap()[0:64, 0:512].rearrange("p (g n) -> p g n", g=4)), [PT1], [KT])

            with k.scope():
                Wq = k.sb("Wq", [128, 8, 1024], BF16)
                for kc in range(8):
                    k.dma("pool", Wq.ap()[:, kc, :], wqkv_d[kc * 128:(kc + 1) * 128, 0:1024], [], [Wq])
                Wo = k.sb("Wo", [64, 16, 1024], BF16)
                for hh in range(16):
                    k.dma("pool", Wo.ap()[:, hh, :], wo_d[hh * 64:(hh + 1) * 64, :], [], [Wo])
                modG = k.sb("modG", [128, D], F32)
                lng = k.sb("lng", [128, D], F32)
                lnb = k.sb("lnb", [128, D], F32)
                self.load_bc(lng, self.ln1_g[i - self.first_layer])
                self.load_bc(lnb, self.ln1_b[i - self.first_layer])
                PS = Rot([k.ps(f"PS{n}", [128, 512], F32) for n in range(2)])
                PO = k.ps("PO", [128, 512], F32)
                PBC = k.ps("PBC", [128, 512], F32)
                QT = k.sb("QT", [64, 2048], BF16)
                onT = k.sb("onT", [64, 2048], BF16)
                pTs = Rot([k.sb(f"pT{n}", [128, 512], BF16) for n in range(3)])
                lnd = k.sb("lnd", [128, 512], F32)
                rd = k.sb("rd", [128, 512], F32)
                osb = k.sb("osb", [64, 512], F32)
                xos = Rot([k.sb(f"xo{n}", [128, D], F32) for n in range(2)])
                st6 = k.sb("st6", [128, 12], F32)
                mvv = k.sb("mvv", [128, 2], F32)
                rstd = k.sb("rstd", [128, 1], F32)
                if not full:
                    mask = k.sb("mask", [128, 2, 128], BF16)
                    k.dma("pool", mask.ap(), self.c_mask, [], [mask])
                    e64 = k.sb("e64", [1, 65], BF16)
                    k.op("dve", lambda e: e.memset(e64.ap(), 0.0), [], [e64])
                    k.op("dve", lambda e: e.memset(e64.ap()[:, 64:65], 1.0), [e64], [e64])
                    sk = k.sb("sk", [1, 16], F32)
                    k.dma("sp", sk.ap(), self.win_sink[j:j + 1, :], [], [sk])
                    k.op("act", lambda e: e.activation(out=sk.ap(), in_=sk.ap(), func=AF.Exp), [sk], [sk])
                    esink = k.sb("esink", [1, 16, 128], BF16)
                    k.op("dve", lambda e: e.tensor_copy(out=esink.ap(), in_=sk.ap().unsqueeze(2).to_broadcast([1, 16, 128])), [sk], [esink])
                g_ty = [-1]
                for t in range(nq):
                    ty = 0 if t < NTL else 1
                    xt, hT = front(t)
                    if g_ty[0] != ty:
                        self.load_mod(i, ty, 2, modG)
                        g_ty[0] = ty
                    for n, P in enumerate((PA, PB)):
                        for kc in range(8):
                            k.op("pe", lambda e: e.matmul(P.ap(), hT.ap()[:, kc * 128:(kc + 1) * 128], Wq.ap()[:, kc, n * 512:(n + 1) * 512],
                                                          start=(kc == 0), stop=(kc == 7)), [hT, Wq], [P], inc=(kc == 7))
                        if full:
                            k.op("act", lambda e: e.copy(out=qs.ap()[:, n * 512:(n + 1) * 512], in_=P.ap()), [P], [qs])
                        else:
                            k.op("dve", lambda e: e.tensor_tensor(out=qs.ap()[:, n * 512:(n + 1) * 512], in0=P.ap(),
                                                                  in1=bias.ap()[:, n * 512:(n + 1) * 512], op=ALU.add), [P, bias], [qs])
                    process_qk(qs, 16, t, qn if full else None, qb)
                    for hh in range(16):
                        PT = PT0 if hh < 8 else PT1
                        k.op("pe", lambda e: e.transpose(PT.ap()[0:64, (hh % 8) * 128:(hh % 8 + 1) * 128], qb.ap()[:, hh * 64:(hh + 1) * 64],
                                                         self.identb.ap()), [qb, self.identb], [PT], inc=(hh % 8 == 7))
                    k.op("act", lambda e: e.copy(out=QT.ap()[:, 0:1024], in_=PT0.ap()[0:64, :]), [PT0], [QT])
                    k.op("act", lambda e: e.copy(out=QT.ap()[:, 1024:2048], in_=PT1.ap()[0:64, :]), [PT1], [QT])
                    if t >= NTL:
                        chunks = [(NTL, None), (NTL + 1, None)]
                    elif full:
                        chunks = [(c, None) for c in range(NT)]
                    else:
                        chunks = []
                        if t > 0:
                            chunks.append((t - 1, 0))
                        chunks.append((t, None))
                        if t < NTL - 1:
                            chunks.append((t + 1, 1))
                        chunks += [(NTL, None), (NTL + 1, None)]
                    for g in range(4):
                        for ci, (jc, m) in enumerate(chunks):
                            S = PS.next()
                            k.op("pe", lambda e: e.matmul(S.ap(), KT.ap()[:, g, jc * 128:(jc + 1) * 128], QT.ap()[:, g * 512:(g + 1) * 512],
                                                          start=True, stop=True), [KT, QT], [S])
                            pT = pTs.next()
                            k.op("act", lambda e: e.activation(out=pT.ap(), in_=S.ap(), func=AF.Exp, scale=0.125), [S], [pT])
                            if m is not None:
                                p3 = pT.ap().rearrange("p (h n) -> p h n", h=4)
                                k.op("dve", lambda e: e.tensor_tensor(out=p3, in0=p3, in1=mask.ap()[:, m, :].unsqueeze(1).to_broadcast([128, 4, 128]),
                                                                      op=ALU.mult), [pT, mask], [pT])
                            lastc = (ci == len(chunks) - 1) and full
                            k.op("pe", lambda e: e.matmul(PO.ap()[0:65, :], V.ap()[:, jc, g, :], pT.ap(), start=(ci == 0), stop=lastc),
                                 [V, pT], [PO])
                        if not full:
                            k.op("pe", lambda e: e.matmul(PO.ap()[0:65, :], e64.ap(), esink.ap()[:, 4 * g:4 * g + 4, :].rearrange("p h n -> p (h n)"),
                                                          start=False, stop=True), [e64, esink], [PO])
                        k.op("act", lambda e: e.activation(out=lnd.ap()[64:65, :], in_=PO.ap()[64:65, :], func=AF.Ln), [PO], [lnd])
                        k.op("act", lambda e: e.activation(out=rd.ap()[64:65, :], in_=lnd.ap()[64:65, :], func=AF.Exp, scale=-1.0), [lnd], [rd])
                        k.op("act", lambda e: e.copy(out=osb.ap(), in_=PO.ap()[0:64, :]), [PO], [osb])
                        k.op("pe", lambda e: e.matmul(PBC.ap()[0:64, :], self.onesf.ap()[64:65, :], rd.ap()[64:65, :], start=True, stop=True),
                             [self.onesf, rd], [PBC])
                        k.op("dve", lambda e: e.tensor_tensor(out=onT.ap()[:, g * 512:(g + 1) * 512], in0=osb.ap(), in1=PBC.ap()[0:64, :],
                                                              op=ALU.mult), [osb, PBC], [onT])
                    for n, P in enumerate((PA, PB)):
                        for hh in range(16):
                            k.op("pe", lambda e: e.matmul(P.ap(), onT.ap()[:, hh * 128:(hh + 1) * 128], Wo.ap()[:, hh, n * 512:(n + 1) * 512],
                                                          start=(hh == 0), stop=(hh == 15)), [onT, Wo], [P], inc=(hh == 15))
                    xo = xos.next()
                    self.resid_ln([(PA.ap(), PA), (PB.ap(), PB)], xt, modG, lng, lnb, tmp2, xo, st6, mvv, rstd)
                    self.store_x(t, xo, last)

    def store_x(self, t, xo, last):
        k = self.k
        if last and (t < NTL or not self.final):
            k.dma("sp", self.out[t * 128:(t + 1) * 128, :], xo.ap(), [xo], [k.out_buf])
        else:
            k.dma("sp", self.X[t * 128:(t + 1) * 128, :], xo.ap(), [xo], [self.Xd[t]])

    def conv_layer(self, i, j, need_ctx, last):
        k = self.k
        nq = NT if need_ctx else NTL
        NTOK = NT * 128
        j = self.jc[j]
        with k.scope():
            PT0 = k.ps("cPT0", [128, 1024], BF16)
            modA = k.sb("cmodA", [128, D], F32)
            modB = k.sb("cmodB", [128, D], F32)
            with k.scope():
                hTall = k.sb("hTall", [128, 8, NTOK], BF16)
                xts = Rot([k.sb(f"cxt{n}", [128, D], F32) for n in range(2)])
                tmp1 = k.sb("ctmp1", [128, D], F32)
                hbs = Rot([k.sb(f"chb{n}", [128, D], BF16) for n in range(2)])
                cur_ty = -1
                for t in range(NT):
                    ty = 0 if t < NTL else 1
                    if cur_ty != ty:
                        self.load_mod(i, ty, 1, modA)
                        self.load_mod(i, ty, 0, modB)
                        cur_ty = ty
                    xt = xts.next()
                    k.dma("sp", xt.ap(), self.X[t * 128:(t + 1) * 128, :], [self.Xd[t]], [xt])
                    k.op("pool", lambda e: e.tensor_tensor(out=tmp1.ap(), in0=xt.ap(), in1=modA.ap(), op=ALU.mult), [xt, modA], [tmp1])
                    hb = hbs.next()
                    k.op("dve", lambda e: e.tensor_tensor(out=hb.ap(), in0=tmp1.ap(), in1=modB.ap(), op=ALU.add), [tmp1, modB], [hb])
                    for c in range(8):
                        k.op("pe", lambda e: e.transpose(PT0.ap()[:, c * 128:(c + 1) * 128], hb.ap()[:, c * 128:(c + 1) * 128], self.identb.ap()),
                             [hb, self.identb], [PT0], inc=(c == 7))
                    k.op("act", lambda e: e.copy(out=hTall.ap()[:, :, t * 128:(t + 1) * 128],
                                                 in_=PT0.ap().rearrange("p (c n) -> p c n", c=8)), [PT0], [hTall])
                cw = k.sb("cw", [128, 8, 3], F32)
                k.dma("sp", cw.ap(), self.conv_wT[j], [], [cw])
                bg = k.sb("cbg", [128, NTOK], F32)
                cx = k.sb("ccx", [128, NTOK + 4], F32)
                yv = k.sb("cyv", [128, NTOK], F32)
                k.op("dve", lambda e: e.memset(cx.ap()[:, 0:1], 0.0), [], [cx])
                k.op("dve", lambda e: e.memset(cx.ap()[:, L + 1:L + 3], 0.0), [cx], [cx])
                k.op("dve", lambda e: e.memset(cx.ap()[:, NTOK + 3:NTOK + 4], 0.0), [cx], [cx])
                mTs = Rot([k.sb(f"cmT{n}", [128, NTOK], BF16) for n in range(1)])
                Wcs = Rot([k.sb(f"cWc{n}", [128, 8, 3, 128], BF16) for n in range(2)])
                csb = k.sb("ccsb", [128, 512], F32)
                PBk = k.ps("cPB", [128, 512], F32)
                PCk = k.ps("cPC", [128, 512], F32)
                PXk = k.ps("cPX", [128, 512], F32)
                groups = [(g * 512, 512) for g in range(8)] + [(L, 256)]
                for jf in range(8):
                    Wc = Wcs.next()
                    for kc in range(8):
                        k.dma("pool", Wc.ap()[:, kc, :, :],
                              self.conv_win[j][kc * 128:(kc + 1) * 128, :].rearrange("p (s n) -> p s n", s=3)[:, :, jf * 128:(jf + 1) * 128],
                              [], [Wc])
                    for (t0, n) in groups:
                        off = 1 + t0 if t0 < L else 3 + t0
                        for s, P in enumerate((PBk, PCk, PXk)):
                            for kc in range(8):
                                k.op("pe", lambda e: e.matmul(P.ap()[:, 0:n], Wc.ap()[:, kc, s, :], hTall.ap()[:, kc, t0:t0 + n],
                                                              start=(kc == 0), stop=(kc == 7)), [Wc, hTall], [P], inc=(kc == 7))
                        k.op("act", lambda e: e.copy(out=bg.ap()[:, t0:t0 + n], in_=PBk.ap()[:, 0:n]), [PBk], [bg])
                        k.op("act", lambda e: e.copy(out=csb.ap()[:, 0:n], in_=PCk.ap()[:, 0:n]), [PCk], [csb])
                        k.op("dve", lambda e: e.tensor_tensor(out=cx.ap()[:, off:off + n], in0=csb.ap()[:, 0:n], in1=PXk.ap()[:, 0:n], op=ALU.mult),
                             [csb, PXk], [cx])
                    mT = mTs.next()
                    for (t0, n) in [(c * 1024, 1024) for c in range(4)] + [(L, 256)]:
                        off = 1 + t0 if t0 < L else 3 + t0
                        k.op("dve", lambda e: e.tensor_scalar(out=yv.ap()[:, t0:t0 + n], in0=cx.ap()[:, off - 1:off - 1 + n],
                                                              scalar1=cw.ap()[:, jf, 0:1], scalar2=None, op0=ALU.mult), [cx, cw], [yv])
                        for s in (1, 2):
                            k.op("dve", lambda e: e.scalar_tensor_tensor(out=yv.ap()[:, t0:t0 + n], in0=cx.ap()[:, off - 1 + s:off - 1 + s + n],
                                                                         scalar=cw.ap()[:, jf, s:s + 1], in1=yv.ap()[:, t0:t0 + n],
                                                                         op0=ALU.mult, op1=ALU.add), [cx, cw, yv], [yv])
                        k.op("pool", lambda e: e.tensor_tensor(out=mT.ap()[:, t0:t0 + n], in0=bg.ap()[:, t0:t0 + n], in1=yv.ap()[:, t0:t0 + n],
                                                               op=ALU.mult), [bg, yv], [mT])
                    k.dma("sp", self.MT[jf], mT.ap(), [mT], [self.MTd])
            with k.scope():
                Wout = k.sb("cWout", [128, 8, D], BF16)
                for kc in range(8):
                    k.dma("pool", Wout.ap()[:, kc, :], self.conv_wout[j][kc * 128:(kc + 1) * 128, :], [], [Wout])
                modG = k.sb("cmodG", [128, D], F32)
                lng = k.sb("clng", [128, D], F32)
                lnb = k.sb("clnb", [128, D], F32)
                self.load_bc(lng, self.ln1_g[i - self.first_layer])
                self.load_bc(lnb, self.ln1_b[i - self.first_layer])
                PA = k.ps("cPA", [128, 512], F32)
                PB = k.ps("cPB2", [128, 512], F32)
                xts = Rot([k.sb(f"dxt{n}", [128, D], F32) for n in range(2)])
                mts = Rot([k.sb(f"dmt{n}", [128, 8, 128], BF16) for n in range(2)])
                xos = Rot([k.sb(f"dxo{n}", [128, D], F32) for n in range(2)])
                u = k.sb("du", [128, D], F32)
                st6 = k.sb("dst6", [128, 12], F32)
                mvv = k.sb("dmvv", [128, 2], F32)
                rstd = k.sb("drstd", [128, 1], F32)
                g_ty = -1
                for t in range(nq):
                    ty = 0 if t < NTL else 1
                    if g_ty != ty:
                        self.load_mod(i, ty, 2, modG)
                        g_ty = ty
                    xt = xts.next()
                    k.dma("sp", xt.ap(), self.X[t * 128:(t + 1) * 128, :], [self.Xd[t]], [xt])
                    mt = mts.next()
                    k.dma("act", mt.ap(), self.MT[:, :, t * 128:(t + 1) * 128].rearrange("j p n -> p j n"), [self.MTd], [mt])
                    for n, P in enumerate((PA, PB)):
                        for jf in range(8):
                            k.op("pe", lambda e: e.matmul(P.ap(), mt.ap()[:, jf, :], Wout.ap()[:, jf, n * 512:(n + 1) * 512],
                                                          start=(jf == 0), stop=(jf == 7)), [mt, Wout], [P], inc=(jf == 7))
                    xo = xos.next()
                    self.resid_ln([(PA.ap(), PA), (PB.ap(), PB)], xt, modG, lng, lnb, u, xo, st6, mvv, rstd)
                    self.store_x(t, xo, last)

    def moe_layer(self, i, need_ctx, last):
        k = self.k
        nt = NT if need_ctx else NTL
        if nt == NT:
            sizes = [7, 7, 7, 7, 6]
        else:
            sizes = [7, 7, 6, 6, 6]
        MAXT = max(sizes)
        groups = []
        a = 0
        for s in sizes:
            groups.append(list(range(a, a + s)))
            a += s
        with k.scope():
            rw = k.sb("rw", [128, 8, NE], F32)
            k.dma("sp", rw.ap(), self.router_w[i - self.first_layer].rearrange("(kc p) e -> p kc e", p=128), [], [rw])
            rb = k.sb("rb", [128, NE], F32)
            self.load_bc(rb, self.router_b[i - self.first_layer])
            bgu = k.sb("bgu", [128, NE, 16], F32)
            k.dma("act", bgu.ap(), self.expert_bguT[i - self.first_layer], [], [bgu])
            Bd = k.sb("Bd", [NE, D], F32)
            k.dma("act", Bd.ap(), self.expert_bdown[i - self.first_layer], [], [Bd])
            hT2 = k.sb("hT2", [128, 8, MAXT * 128], BF16)
            yacc = k.sb("yacc", [128, MAXT, D], F32)
            GTall = k.sb("GTall", [NE, MAXT * 128], F32)
            actT = k.sb("actT", [128, 8, MAXT * 128], BF16)
            gbcs = Rot([k.sb(f"gbc{n}", [128, MAXT * 128], F32) for n in range(1)])
            Wgs = Rot([k.sb(f"Wg{n}", [128, 8, 2 * D], BF16) for n in range(2)])
            Wds = Rot([k.sb(f"Wd{n}", [128, 8, D], BF16) for n in range(1)])
            modA = k.sb("mmodA", [128, D], F32)
            modB = k.sb("mmodB", [128, D], F32)
            modG = modA
            lng = modB
            lnb = k.sb("mlnb", [128, D], F32)
            self.load_bc(lnb, self.ln2_b[i - self.first_layer])
            xts = Rot([k.sb(f"mxt{n}", [128, D], F32) for n in range(2)])
            tmp1 = k.sb("mtmp1", [128, D], F32)
            hTf = k.sb("mhTf", [128, D], F32)
            xos = Rot([k.sb(f"mxo{n}", [128, D], F32) for n in range(1)])
            st6 = k.sb("mst6", [128, 12], F32)
            mvv = k.sb("mmvv", [128, 2], F32)
            rstd = k.sb("mrstd", [128, 1], F32)
            lg = k.sb("lg", [128, NE], F32)
            top8 = k.sb("top8", [128, 8], F32)
            msk = k.sb("msk", [128, NE], F32)
            nmx = k.sb("nmx", [128, 1], F32)
            ssum = k.sb("ssum", [128, 1], F32)
            Gt = k.sb("Gt", [128, 128], F32)
            k.op("dve", lambda e: e.memset(Gt.ap(), 0.0), [], [Gt])
            GTs = Rot([k.sb(f"GTs{n}", [NE, 128], F32) for n in range(2)])
            g1s = Rot([k.sb(f"g1_{n}", [128, 512], F32) for n in range(2)])
            sgs = Rot([k.sb(f"sg_{n}", [128, 512], F32) for n in range(2)])
            u1s = Rot([k.sb(f"u1_{n}", [128, 512], F32) for n in range(2)])
            t1s = Rot([k.sb(f"t1_{n}", [128, 512], F32) for n in range(2)])
            P0 = k.ps("mP0", [128, 512], F32)
            P1 = k.ps("mP1", [128, 512], F32)
            PGs = Rot([k.ps(f"mPG{n}", [128, 512], F32) for n in range(2)])
            PUs = Rot([k.ps(f"mPU{n}", [128, 512], F32) for n in range(2)])
            PYs = Rot([k.ps(f"mPY{n}", [128, 512], F32) for n in range(2)])
            cur = {"a": -1, "g": -1}
            import os
            STOP = int(os.environ.get("MOE_STOP", 99))
            for grp in groups[:int(os.environ.get('MOE_NGRP', 99))]:
                ntg = len(grp)
                ntok = ntg * 128
                tok0 = grp[0] * 128
                for a, t in enumerate(grp):
                    ty = 0 if t < NTL else 1
                    if cur["a"] != ty:
                        self.load_mod(i, ty, 4, modA)
                        self.load_mod(i, ty, 3, modB)
                        cur["a"] = ty
                    xt = xts.next()
                    k.dma("sp", xt.ap(), self.X[t * 128:(t + 1) * 128, :], [self.Xd[t]], [xt])
                    k.op("dve", lambda e: e.tensor_tensor(out=tmp1.ap(), in0=xt.ap(), in1=modA.ap(), op=ALU.mult), [xt, modA], [tmp1])
                    k.op("dve", lambda e: e.tensor_tensor(out=tmp1.ap(), in0=tmp1.ap(), in1=modB.ap(), op=ALU.add), [tmp1, modB], [tmp1])
                    if STOP <= 1:
                        continue
                    for c in range(8):
                        P = P0 if c < 4 else P1
                        k.op("pe", lambda e: e.transpose(P.ap()[:, (c % 4) * 128:(c % 4 + 1) * 128], tmp1.ap()[:, c * 128:(c + 1) * 128], self.identf.ap()),
                             [tmp1, self.identf], [P], inc=(c % 4 == 3))
                    if STOP <= 2:
                        continue
                    for hh, P in enumerate((P0, P1)):
                        S3 = os.environ.get("MOE_S3", "ab")
                        if "a" in S3:
                            k.op("dve", lambda e: e.tensor_copy(out=hTf.ap()[:, hh * 512:(hh + 1) * 512], in_=P.ap()), [P], [hTf])
                        if "b" in S3:
                            k.op("dve", lambda e: e.tensor_copy(out=hT2.ap()[:, hh * 4:(hh + 1) * 4, a * 128:(a + 1) * 128],
                                                                in_=P.ap().rearrange("p (c n) -> p c n", c=4)), [P], [hT2])
                        if "c" in S3:
                            for c4 in range(4):
                                k.op("dve", lambda e: e.tensor_copy(out=hT2.ap()[:, hh * 4 + c4, a * 128:(a + 1) * 128],
                                                                    in_=P.ap()[:, c4 * 128:(c4 + 1) * 128]), [P], [hT2])
                    if STOP <= 3:
                        continue
                    PR = PGs.next()
                    for kc in range(8):
                        k.op("pe", lambda e: e.matmul(PR.ap()[:, 0:NE], hTf.ap()[:, kc * 128:(kc + 1) * 128], rw.ap()[:, kc, :],
                                                      start=(kc == 0), stop=(kc == 7)), [hTf, rw], [PR], inc=(kc == 7))
                    if STOP <= 4:
                        continue
                    k.op("dve", lambda e: e.tensor_tensor(out=lg.ap(), in0=PR.ap()[:, 0:NE], in1=rb.ap(), op=ALU.add), [PR, rb], [lg])
                    k.op("dve", lambda e: e.max(out=top8.ap(), in_=lg.ap()), [lg], [top8])
                    k.op("dve", lambda e: e.tensor_scalar(out=msk.ap(), in0=lg.ap(), scalar1=top8.ap()[:, 3:4], scalar2=None, op0=ALU.is_ge),
                         [lg, top8], [msk])
                    k.op("dve", lambda e: e.tensor_scalar(out=nmx.ap(), in0=top8.ap()[:, 0:1], scalar1=-1.0, scalar2=None, op0=ALU.mult),
                         [top8], [nmx])
                    k.op("act", lambda e: e.activation(out=lg.ap(), in_=lg.ap(), func=AF.Exp, bias=nmx.ap(), scale=1.0), [lg, nmx], [lg])
                    k.op("dve", lambda e: e.tensor_tensor(out=lg.ap(), in0=lg.ap(), in1=msk.ap(), op=ALU.mult), [lg, msk], [lg])
                    k.op("dve", lambda e: e.tensor_reduce(out=ssum.ap(), in_=lg.ap(), axis=AX.X, op=ALU.add), [lg], [ssum])
                    k.op("dve", lambda e: e.reciprocal(out=ssum.ap(), in_=ssum.ap()), [ssum], [ssum])
                    k.op("dve", lambda e: e.tensor_scalar(out=Gt.ap()[:, 0:NE], in0=lg.ap(), scalar1=ssum.ap(), scalar2=None, op0=ALU.mult),
                         [lg, ssum], [Gt])
                    if STOP <= 5:
                        continue
                    PG_ = PUs.next()
                    k.op("pe", lambda e: e.transpose(PG_.ap()[:, 0:128], Gt.ap(), self.identf.ap()), [Gt, self.identf], [PG_])
                    GT_ = GTs.next()
                    k.op("dve", lambda e: e.tensor_copy(out=GT_.ap(), in_=PG_.ap()[0:NE, 0:128]), [PG_], [GT_])
                    k.op("dve", lambda e: e.tensor_copy(out=GTall.ap()[:, a * 128:(a + 1) * 128], in_=GT_.ap()), [GT_], [GTall])
                    k.dma("sp", self.GTD[:, t * 128:(t + 1) * 128], GT_.ap(), [GT_], [self.GTDd])
                    if STOP <= 6:
                        continue
                    for n in range(2):
                        PY = PYs.next()
                        k.op("pe", lambda e: e.matmul(PY.ap(), GTall.ap()[:, a * 128:(a + 1) * 128], Bd.ap()[:, n * 512:(n + 1) * 512],
                                                      start=True, stop=True), [GTall, Bd], [PY])
                        k.op("act", lambda e: e.copy(out=yacc.ap()[:, a, n * 512:(n + 1) * 512], in_=PY.ap()), [PY], [yacc])
                if STOP < 99:
                    continue
                tgs = []
                o = 0
                while o < ntok:
                    n = min(512, ntok - o)
                    tgs.append((o, n))
                    o += n
                import os
                for ex in range(int(os.environ.get('MOE_NEXP', NE))):
                    Wg = Wgs.next()
                    Wd = Wds.next()
                    for kc in range(8):
                        k.dma("pool", Wg.ap()[:, kc, :], self.expert_wgu[i - self.first_layer, ex, kc * 128:(kc + 1) * 128, :], [], [Wg])
                    for kc in range(8):
                        k.dma("pool", Wd.ap()[:, kc, :], self.expert_wdown[i - self.first_layer, ex, kc * 128:(kc + 1) * 128, :], [], [Wd])
                    gbc = gbcs.next()
                    k.dma("sp", gbc.ap()[:, 0:ntok], self.GTD[ex, tok0:tok0 + ntok].partition_broadcast(128), [self.GTDd], [gbc])
                    for f in range(8):
                        for (o, n) in tgs:
                            PG = PGs.next()
                            PU = PUs.next()
                            for (P, cb) in ((PG, f * 128), (PU, D + f * 128)):
                                for kc in range(8):
                                    k.op("pe", lambda e: e.matmul(P.ap()[:, 0:n], Wg.ap()[:, kc, cb:cb + 128], hT2.ap()[:, kc, o:o + n],
                                                                  start=(kc == 0), stop=(kc == 7)), [Wg, hT2], [P], inc=(kc == 7))
                            g1 = g1s.next()
                            sg = sgs.next()
                            u1 = u1s.next()
                            t1 = t1s.next()
                            k.op("dve", lambda e: e.tensor_scalar(out=g1.ap()[:, 0:n], in0=PG.ap()[:, 0:n], scalar1=bgu.ap()[:, ex, f:f + 1],
                                                                  scalar2=7.0, op0=ALU.add, op1=ALU.min), [PG, bgu], [g1])
                            k.op("act", lambda e: e.activation(out=sg.ap()[:, 0:n], in_=g1.ap()[:, 0:n], func=AF.Sigmoid, scale=1.702), [g1], [sg])
                            k.op("dve", lambda e: e.tensor_scalar(out=u1.ap()[:, 0:n], in0=PU.ap()[:, 0:n], scalar1=bgu.ap()[:, ex, 8 + f:9 + f],
                                                                  scalar2=7.0, op0=ALU.add, op1=ALU.min), [PU, bgu], [u1])
                            k.op("dve", lambda e: e.tensor_scalar(out=u1.ap()[:, 0:n], in0=u1.ap()[:, 0:n], scalar1=-7.0, scalar2=1.0,
                                                                  op0=ALU.max, op1=ALU.add), [u1], [u1])
                            k.op("pool", lambda e: e.tensor_tensor(out=t1.ap()[:, 0:n], in0=g1.ap()[:, 0:n], in1=sg.ap()[:, 0:n], op=ALU.mult),
                                 [g1, sg], [t1])
                            k.op("dve", lambda e: e.tensor_tensor(out=t1.ap()[:, 0:n], in0=t1.ap()[:, 0:n], in1=u1.ap()[:, 0:n], op=ALU.mult),
                                 [t1, u1], [t1])
                            k.op("dve", lambda e: e.tensor_tensor(out=actT.ap()[:, f, o:o + n], in0=t1.ap()[:, 0:n], in1=gbc.ap()[:, o:o + n],
                                                                  op=ALU.mult), [t1, gbc], [actT])
                    for a in range(ntg):
                        for n in range(2):
                            PY = PYs.next()
                            for f in range(8):
                                k.op("pe", lambda e: e.matmul(PY.ap(), actT.ap()[:, f, a * 128:(a + 1) * 128], Wd.ap()[:, f, n * 512:(n + 1) * 512],
                                                              start=(f == 0), stop=(f == 7)), [actT, Wd], [PY], inc=(f == 7))
                            ysl = yacc.ap()[:, a, n * 512:(n + 1) * 512]
                            k.op("dve", lambda e: e.tensor_tensor(out=ysl, in0=PY.ap(), in1=ysl, op=ALU.add), [PY, yacc], [yacc])
                cur["a"] = -1
                cur["g"] = -1
                self.load_bc(lng, self.ln2_g[i - self.first_layer])
                for a, t in enumerate(grp):
                    ty = 0 if t < NTL else 1
                    if cur["g"] != ty:
                        self.load_mod(i, ty, 5, modG)
                        cur["g"] = ty
                    xt = xts.next()
                    k.dma("sp", xt.ap(), self.X[t * 128:(t + 1) * 128, :], [self.Xd[t]], [xt])
                    xo = xos.next()
                    self.resid_ln([(yacc.ap()[:, a, 0:512], yacc), (yacc.ap()[:, a, 512:1024], yacc)], xt, modG, lng, lnb, tmp1, xo, st6, mvv, rstd)
                    self.store_x(t, xo, last)

    def build(self):
        k = self.k
        self.consts()
        self.prologue()
        for i in range(self.first_layer, self.first_layer + self.n_layers):
            kind, j = i % 3, i // 3
            need_ctx = i < DEPTH - 1
            last = (i == self.first_layer + self.n_layers - 1)
            if self.debug != "moe_only":
                if kind == 1:
                    self.conv_layer(i, j, need_ctx, last and self.debug == "mixer_only")
                else:
                    self.attention_layer(i, kind == 2, j, need_ctx, last and self.debug == "mixer_only")
            if self.debug != "mixer_only":
                self.moe_layer(i, need_ctx, last)
        k.finish()
        return self.nc


def _rope_tables():
    n_freq = 16
    inv = (10000.0 ** (-np.arange(n_freq, dtype=np.float32) / n_freq)).astype(np.float32)
    tok = np.arange(L)
    row = (tok // 64).astype(np.float32)
    col = (tok % 64).astype(np.float32)
    ang = np.stack([row[:, None] * inv, col[:, None] * inv], axis=1).astype(np.float32)
    cos = np.cos(ang).astype(np.float32).reshape(NTL, 128, 32).transpose(1, 0, 2)
    sin = np.sin(ang).astype(np.float32).reshape(NTL, 128, 32).transpose(1, 0, 2)
    return np.ascontiguousarray(cos), np.ascontiguousarray(sin)


def make_in_maps(inputs, cores, first_layer=0, n_layers=DEPTH, xs=None):
    f = lambda a: np.ascontiguousarray(np.asarray(a, dtype=np.float32))
    layers = list(range(first_layer, first_layer + n_layers))
    cos, sin = _rope_tables()
    p = np.arange(128)
    mask = np.zeros((128, 2, 128), np.float32)
    mask[:, 0, :] = (p[:, None] >= p[None, :])
    mask[:, 1, :] = (p[:, None] <= p[None, :])
    shared = {"c_ident": np.eye(128, dtype=np.float32), "c_cos": cos, "c_sin": sin, "c_mask": mask}
    sl = slice(first_layer, first_layer + n_layers)
    for name in ("mod_w", "mod_b", "ln1_g", "ln1_b", "ln2_g", "ln2_b", "router_w", "router_b",
                 "expert_wgu", "expert_wdown", "expert_bdown"):
        shared[name] = f(inputs[name][sl])
    shared["expert_bguT"] = np.ascontiguousarray(f(inputs["expert_bgu"][sl]).reshape(n_layers, NE, 16, 128).transpose(0, 3, 1, 2))

    def pick(names, js, extra=None):
        js = sorted(js)
        for name in names:
            a = np.asarray(inputs[name])
            if js:
                shared[name] = f(a[js])
            else:
                shared[name] = np.zeros((1,) + a.shape[1:], np.float32)

    pick(("win_wqkv", "win_bqkv", "win_sink", "win_wo"), {i // 3 for i in layers if i % 3 == 0})
    jc = sorted({i // 3 for i in layers if i % 3 == 1})
    pick(("conv_win", "conv_wout"), jc)
    cw = f(inputs["conv_w"])
    if jc:
        shared["conv_wT"] = np.ascontiguousarray(cw[jc].reshape(len(jc), 3, 8, 128).transpose(0, 3, 2, 1))
    else:
        shared["conv_wT"] = np.zeros((1, 128, 8, 3), np.float32)
    pick(("full_wqkv", "full_qnorm", "full_knorm", "full_wo"), {i // 3 for i in layers if i % 3 == 2})
    x = f(inputs["x"])
    ctx = f(inputs["ctx"])
    c = f(inputs["c"])
    c_ctx = f(inputs["c_ctx"])
    maps = []
    for n, b in enumerate(cores):
        m = dict(shared)
        if xs is None:
            m["x"] = x[b]
            m["ctx"] = ctx[b]
        else:
            m["x"] = np.ascontiguousarray(xs[n][:L])
            m["ctx"] = np.ascontiguousarray(xs[n][L:])
        cc = np.stack([c[b].reshape(8, 128), c_ctx.reshape(8, 128)], axis=-1)
        m["ccT"] = np.ascontiguousarray(cc.transpose(1, 0, 2).reshape(128, 16))
        maps.append(m)
    return maps


LAUNCH_SPLIT = [(0, 4)]


def kernel(**inputs):
    cores = list(range(8))
    xs = None
    for li, (first, nl) in enumerate(LAUNCH_SPLIT):
        final = (li == len(LAUNCH_SPLIT) - 1)
        prog = Prog(n_layers=nl, first_layer=first, final=final)
        nc = prog.build()
        maps = make_in_maps(inputs, cores, first, nl, xs)
        res = run_bass_kernel_spmd(nc, maps, core_ids=cores)
        if final:
            return np.stack([np.asarray(r["out"], dtype=np.float32) for r in res.results], axis=0)
        xs = [np.asarray(r["xs"], dtype=np.float32) for r in res.results]
```

```python
from contextlib import ExitStack, contextmanager
import numpy as np
import concourse.bass as bass
import concourse.mybir as mybir
from concourse.bass_utils import run_bass_kernel_spmd

F32 = mybir.dt.float32
BF16 = mybir.dt.bfloat16
I32 = mybir.dt.int32
AF = mybir.ActivationFunctionType
ALU = mybir.AluOpType
AX = mybir.AxisListType


class Buf:
    __slots__ = ("name", "h", "last_w", "readers", "epoch")

    def __init__(self, name, h=None):
        self.name = name
        self.h = h
        self.last_w = None
        self.readers = {}
        self.epoch = 0

    def ap(self):
        return self.h[:]

    def __getitem__(self, idx):
        return self.h[idx]


class K:
    NSLOT = 4

    def __init__(self, nc):
        self.nc = nc
        self.stack = ExitStack()
        self.eng = {"pe": nc.tensor, "act": nc.scalar, "dve": nc.vector, "pool": nc.gpsimd, "sp": nc.sync}
        self.NSETS = 5
        self.semsets = [{} for _ in range(self.NSETS)]
        self.count = {}
        self.seen = {e: {} for e in self.eng}
        self.slots = {}
        self.slot_rr = {}
        for e in self.eng:
            for si in range(self.NSETS):
                self.semsets[si][e] = self.stack.enter_context(nc.semaphore(f"s{si}_{e}"))
            self.count[e] = 0
        for q in ("sp", "act", "pool"):
            self.slots[q] = []
            for i in range(self.NSLOT):
                key = f"d_{q}{i}"
                for si in range(self.NSETS):
                    self.semsets[si][key] = self.stack.enter_context(nc.semaphore(f"s{si}_{key}"))
                self.count[key] = 0
                self.slots[q].append(key)
            self.slot_rr[q] = 0
        self.sems = self.semsets[0]
        self.out_buf = Buf("out")
        self.scopes = [self.stack]
        self.n_inst = 0
        self.epoch = 0
        self.round = 0

    def _uniq(self, name):
        self._nuniq = getattr(self, "_nuniq", 0) + 1
        return f"{name}_{self._nuniq}"

    def sb(self, name, shape, dtype):
        h = self.scopes[-1].enter_context(self.nc.sbuf_tensor(self._uniq("sb_" + name), list(shape), dtype))
        return Buf(name, h)

    def ps(self, name, shape, dtype):
        h = self.scopes[-1].enter_context(self.nc.psum_tensor(self._uniq("ps_" + name), list(shape), dtype))
        return Buf(name, h)

    @contextmanager
    def scope(self):
        st = ExitStack()
        self.scopes.append(st)
        try:
            yield
        finally:
            self.barrier()
            self.scopes.pop()
            st.close()

    def _wait(self, e, key, val):
        if val <= 0:
            return
        assert val < 60000, (key, val)
        if self.seen[e].get(key, 0) >= val:
            return
        self.eng[e].wait_ge(self.sems[key], val)
        self.seen[e][key] = val
        self.n_inst += 1

    def _deps(self, e, reads, writes):
        need = {}
        for b in list(reads) + list(writes):
            if b.epoch != self.epoch:
                b.epoch = self.epoch
                b.last_w = None
                b.readers = {}
        for b in reads:
            if b.last_w is not None:
                k_, v = b.last_w
                need[k_] = max(need.get(k_, 0), v)
        for b in writes:
            if b.last_w is not None:
                k_, v = b.last_w
                need[k_] = max(need.get(k_, 0), v)
            for k_, v in b.readers.items():
                need[k_] = max(need.get(k_, 0), v)
        return need

    def _record(self, ev, reads, writes):
        k_, v = ev
        for b in reads:
            if b.readers.get(k_, 0) < v:
                b.readers[k_] = v
        for b in writes:
            b.last_w = ev
            b.readers = {}

    def op(self, e, fn, reads=(), writes=(), inc=True):
        need = self._deps(e, reads, writes)
        for k_, v in need.items():
            if e == "pe" and k_ == "pe":
                continue
            self._wait(e, k_, v)
        ins = fn(self.eng[e])
        self.n_inst += 1
        if inc:
            ins.then_inc(self.sems[e], 1)
            self.count[e] += 1
            ev = (e, self.count[e])
        else:
            ev = (e, self.count[e] + 1)
        self._record(ev, reads, writes)
        return ev

    def dma(self, q, out, in_, reads=(), writes=(), **kw):
        need = self._deps(q, reads, writes)
        for k_, v in need.items():
            self._wait(q, k_, v)
        i = self.slot_rr[q]
        self.slot_rr[q] = (i + 1) % self.NSLOT
        key = self.slots[q][i]
        self._wait(q, key, self.count[key])
        ins = self.eng[q].dma_start(out=out, in_=in_, **kw)
        ins.then_inc(self.sems[key], 16)
        self.n_inst += 1
        self.count[key] += 16
        ev = (key, self.count[key])
        self._record(ev, reads, writes)
        return ev

    def barrier(self, reset=True):
        import os
        if os.environ.get("NORESET"):
            reset = False
        for e in self.eng:
            for key in self.sems:
                if key == e:
                    continue
                self._wait(e, key, self.count[key])
        if not reset:
            return
        if max(self.count.values()) < 12000:
            return
        self.round += 1
        assert self.round < self.NSETS
        self.sems = self.semsets[self.round]
        for key in self.count:
            self.count[key] = 0
        self.seen = {e: {} for e in self.eng}
        self.epoch += 1

    def finish(self):
        self.barrier(reset=False)
        self.stack.close()


D = 1024
L = 4096
CL = 256
NTL = 32
NT = 34
DEPTH = 4
NE = 32
ALPHA = (2.0 * DEPTH) ** 0.25
LN_EPS = 1e-5
RMS_EPS = 1e-6


class Rot:
    def __init__(self, bufs):
        self.bufs = list(bufs)
        self.i = 0

    def next(self):
        b = self.bufs[self.i % len(self.bufs)]
        self.i += 1
        return b


class Prog:
    def __init__(self, n_layers=DEPTH, first_layer=0, debug=False, final=True):
        self.n_layers = n_layers
        self.first_layer = first_layer
        self.final = final
        nc = bass.Bass("TRN2", target_bir_lowering=False)
        self.nc = nc
        self.k = K(nc)

        def din(name, shape, dt=F32):
            return nc.dram_tensor(name, list(shape), dt, kind="ExternalInput").ap()

        layers = list(range(first_layer, first_layer + n_layers))
        nl = n_layers
        self.jw = {j: n for n, j in enumerate(sorted({i // 3 for i in layers if i % 3 == 0}))}
        self.jc = {j: n for n, j in enumerate(sorted({i // 3 for i in layers if i % 3 == 1}))}
        self.jf = {j: n for n, j in enumerate(sorted({i // 3 for i in layers if i % 3 == 2}))}
        nw, ncv, nf = max(1, len(self.jw)), max(1, len(self.jc)), max(1, len(self.jf))
        self.x = din("x", [L, D])
        self.ctx = din("ctx", [CL, D])
        self.ccT = din("ccT", [128, 16])
        self.mod_w = din("mod_w", [nl, D, 6 * D])
        self.mod_b = din("mod_b", [nl, 6 * D])
        self.ln1_g = din("ln1_g", [nl, D])
        self.ln1_b = din("ln1_b", [nl, D])
        self.ln2_g = din("ln2_g", [nl, D])
        self.ln2_b = din("ln2_b", [nl, D])
        self.win_wqkv = din("win_wqkv", [nw, D, 1536])
        self.win_bqkv = din("win_bqkv", [nw, 1536])
        self.win_sink = din("win_sink", [nw, 16])
        self.win_wo = din("win_wo", [nw, D, D])
        self.conv_win = din("conv_win", [ncv, D, 3 * D])
        self.conv_wT = din("conv_wT", [ncv, 128, 8, 3])
        self.conv_wout = din("conv_wout", [ncv, D, D])
        self.full_wqkv = din("full_wqkv", [nf, D, 1536])
        self.full_qnorm = din("full_qnorm", [nf, 64])
        self.full_knorm = din("full_knorm", [nf, 64])
        self.full_wo = din("full_wo", [nf, D, D])
        self.router_w = din("router_w", [nl, D, NE])
        self.router_b = din("router_b", [nl, NE])
        self.expert_wgu = din("expert_wgu", [nl, NE, D, 2 * D])
        self.expert_bguT = din("expert_bguT", [nl, 128, NE, 16])
        self.expert_wdown = din("expert_wdown", [nl, NE, D, D])
        self.expert_bdown = din("expert_bdown", [nl, NE, D])
        self.c_ident = din("c_ident", [128, 128])
        self.c_cos = din("c_cos", [128, NTL, 32])
        self.c_sin = din("c_sin", [128, NTL, 32])
        self.c_mask = din("c_mask", [128, 2, 128])
        if final:
            self.out = nc.dram_tensor("out", [L, D], F32, kind="ExternalOutput").ap()
        else:
            self.out = nc.dram_tensor("xs", [NT * 128, D], F32, kind="ExternalOutput").ap()
        self.X = nc.dram_tensor("Xs", [NT * 128, D], F32, kind="Internal").ap()
        self.MODV = nc.dram_tensor("MODV", [4, 2, 6 * D], F32, kind="Internal").ap()
        self.GTD = nc.dram_tensor("GTD", [NE, NT * 128], F32, kind="Internal").ap()
        self.MT = nc.dram_tensor("MTs", [8, 128, NT * 128], BF16, kind="Internal").ap()
        self.Xd = [Buf(f"X{t}") for t in range(NT)]
        self.MODVd = Buf("MODVd")
        self.GTDd = Buf("GTDd")
        self.MTd = Buf("MTd")
        self.debug = debug

    def consts(self):
        k = self.k
        self.identf = k.sb("identf", [128, 128], F32)
        k.dma("sp", self.identf.ap(), self.c_ident, [], [self.identf])
        self.identb = k.sb("identb", [128, 128], BF16)
        k.dma("pool", self.identb.ap(), self.c_ident, [], [self.identb])
        self.eps_ln = k.sb("eps_ln", [128, 1], F32)
        k.op("dve", lambda e: e.memset(self.eps_ln.ap(), LN_EPS), [], [self.eps_ln])
        self.eps_rms = k.sb("eps_rms", [128, 1], F32)
        k.op("dve", lambda e: e.memset(self.eps_rms.ap(), RMS_EPS), [], [self.eps_rms])
        self.onesf = k.sb("onesf", [128, 64], F32)
        k.op("dve", lambda e: e.memset(self.onesf.ap(), 1.0), [], [self.onesf])

    def load_bc(self, buf, src_row, srcbuf=None, q="sp"):
        n = buf.ap().shape[0]
        self.k.dma(q, buf.ap(), src_row.partition_broadcast(n), [srcbuf] if srcbuf else [], [buf])

    def prologue(self):
        k = self.k
        for t in range(NT):
            src = self.x[t * 128:(t + 1) * 128, :] if t < NTL else self.ctx[(t - NTL) * 128:(t - NTL + 1) * 128, :]
            k.dma("sp" if t % 2 else "act", self.X[t * 128:(t + 1) * 128, :], src, [], [self.Xd[t]])
        with k.scope():
            cc = k.sb("cc", [128, 16], F32)
            k.dma("sp", cc.ap(), self.ccT, [], [cc])
            sT = k.sb("sT", [128, 16], F32)
            k.op("act", lambda e: e.activation(out=sT.ap(), in_=cc.ap(), func=AF.Silu), [cc], [sT])
            wb = Rot([k.sb(f"mw{i}", [128, 8, 512], F32) for i in range(2)])
            pp = Rot([k.ps(f"mps{i}", [2, 512], F32) for i in range(2)])
            mv = k.sb("mv", [2, 6 * D], F32)
            mb = k.sb("mb", [2, 6 * D], F32)
            for i in range(self.first_layer, self.first_layer + self.n_layers):
                self.load_bc(mb, self.mod_b[i - self.first_layer], q="act")
                for n in range(12):
                    w = wb.next()
                    p = pp.next()
                    k.dma("sp", w.ap(), self.mod_w[i - self.first_layer][:, n * 512:(n + 1) * 512].rearrange("(kc p) n -> p kc n", p=128), [], [w])
                    for kc in range(8):
                        k.op("pe", lambda e: e.matmul(p.ap(), sT.ap()[:, 2 * kc:2 * kc + 2], w.ap()[:, kc, :],
                                                      start=(kc == 0), stop=(kc == 7)), [sT, w], [p], inc=(kc == 7))
                    k.op("dve", lambda e: e.tensor_tensor(out=mv.ap()[:, n * 512:(n + 1) * 512], in0=p.ap(),
                                                          in1=mb.ap()[:, n * 512:(n + 1) * 512], op=ALU.add), [p, mb], [mv])
                for seg in (1, 4):
                    sl = mv.ap()[:, seg * D:(seg + 1) * D]
                    k.op("dve", lambda e: e.tensor_scalar(out=sl, in0=sl, scalar1=1.0, scalar2=None, op0=ALU.add), [mv], [mv])
                k.dma("sp", self.MODV[i], mv.ap(), [mv], [self.MODVd])

    def load_mod(self, i, ty, seg, buf):
        self.load_bc(buf, self.MODV[i, ty, seg * D:(seg + 1) * D], self.MODVd)

    def resid_ln(self, yp, xt, gbc, lng, lnb, u, xo, st6, mvv, rstd):
        k = self.k
        for n in range(2):
            ap_, b_ = yp[n]
            k.op("dve", lambda e: e.tensor_tensor(out=u.ap()[:, n * 512:(n + 1) * 512], in0=ap_,
                                                  in1=gbc.ap()[:, n * 512:(n + 1) * 512], op=ALU.mult), [b_, gbc], [u])
        k.op("dve", lambda e: e.scalar_tensor_tensor(out=u.ap(), in0=xt.ap(), scalar=ALPHA, in1=u.ap(),
                                                     op0=ALU.mult, op1=ALU.add), [xt, u], [u])
        for n in range(2):
            k.op("dve", lambda e: e.bn_stats(out=st6.ap()[:, n * 6:(n + 1) * 6], in_=u.ap()[:, n * 512:(n + 1) * 512]), [u], [st6])
        k.op("dve", lambda e: e.bn_aggr(out=mvv.ap(), in_=st6.ap()), [st6], [mvv])
        k.op("act", lambda e: e.activation(out=rstd.ap(), in_=mvv.ap()[:, 1:2], func=AF.Sqrt, bias=self.eps_ln.ap(), scale=1.0),
             [mvv, self.eps_ln], [rstd])
        k.op("dve", lambda e: e.reciprocal(out=rstd.ap(), in_=rstd.ap()), [rstd], [rstd])
        k.op("dve", lambda e: e.tensor_scalar(out=u.ap(), in0=u.ap(), scalar1=mvv.ap()[:, 0:1], scalar2=rstd.ap(),
                                              op0=ALU.subtract, op1=ALU.mult), [u, mvv, rstd], [u])
        k.op("pool", lambda e: e.tensor_tensor(out=u.ap(), in0=u.ap(), in1=lng.ap(), op=ALU.mult), [u, lng], [u])
        k.op("pool", lambda e: e.tensor_tensor(out=xo.ap(), in0=u.ap(), in1=lnb.ap(), op=ALU.add), [u, lnb], [xo])

    def attention_layer(self, i, full, j, need_ctx, last):
        k = self.k
        nq = NT if need_ctx else NTL
        j = self.jf[j] if full else self.jw[j]
        wqkv_d = self.full_wqkv[j] if full else self.win_wqkv[j]
        wo_d = self.full_wo[j] if full else self.win_wo[j]
        with k.scope():
            KT = k.sb("KT", [64, 4, NT * 128], BF16)
            V = k.sb("V", [128, NT, 4, 65], BF16)
            k.op("pool", lambda e: e.memset(V.ap(), 1.0), [], [V])
            COS = k.sb("COS", [128, NTL, 32], F32)
            SIN = k.sb("SIN", [128, NTL, 32], F32)
            k.dma("sp", COS.ap(), self.c_cos, [], [COS])
            k.dma("act", SIN.ap(), self.c_sin, [], [SIN])
            modA = k.sb("modA", [128, D], F32)
            modB = k.sb("modB", [128, D], F32)
            PT0 = k.ps("PT0", [128, 1024], BF16)
            PT1 = k.ps("PT1", [128, 1024], BF16)
            PA = k.ps("PA", [128, 512], F32)
            PB = k.ps("PB", [128, 512], F32)
            xts = Rot([k.sb(f"xt{n}", [128, D], F32) for n in range(2)])
            tmp1 = k.sb("tmp1", [128, D], F32)
            tmp2 = k.sb("tmp2", [128, D], F32)
            hbs = Rot([k.sb(f"hb{n}", [128, D], BF16) for n in range(2)])
            hTs = Rot([k.sb(f"hT{n}", [128, D], BF16) for n in range(2)])
            qs = k.sb("qs", [128, D], F32)
            qb = k.sb("qb", [128, D], BF16)
            ms = k.sb("ms", [128, 16], F32)
            if full:
                qn = k.sb("qn", [128, 64], F32)
                kn = k.sb("kn", [128, 64], F32)
                self.load_bc(qn, self.full_qnorm[j])
                self.load_bc(kn, self.full_knorm[j])
            else:
                bias = k.sb("bias", [128, 1536], F32)
                self.load_bc(bias, self.win_bqkv[j])
            cur_ty = [-1]

            def front(t):
                ty = 0 if t < NTL else 1
                if cur_ty[0] != ty:
                    self.load_mod(i, ty, 1, modA)
                    self.load_mod(i, ty, 0, modB)
                    cur_ty[0] = ty
                xt = xts.next()
                k.dma("sp", xt.ap(), self.X[t * 128:(t + 1) * 128, :], [self.Xd[t]], [xt])
                k.op("pool", lambda e: e.tensor_tensor(out=tmp1.ap(), in0=xt.ap(), in1=modA.ap(), op=ALU.mult), [xt, modA], [tmp1])
                hb = hbs.next()
                k.op("dve", lambda e: e.tensor_tensor(out=hb.ap(), in0=tmp1.ap(), in1=modB.ap(), op=ALU.add), [tmp1, modB], [hb])
                for c in range(8):
                    k.op("pe", lambda e: e.transpose(PT0.ap()[:, c * 128:(c + 1) * 128], hb.ap()[:, c * 128:(c + 1) * 128], self.identb.ap()),
                         [hb, self.identb], [PT0], inc=(c == 7))
                hT = hTs.next()
                k.op("act", lambda e: e.copy(out=hT.ap(), in_=PT0.ap()), [PT0], [hT])
                return xt, hT

            def process_qk(src, H, t, gn, dst):
                W = H * 64
                s2 = src.ap()[:, 0:W]
                s3 = s2.rearrange("p (h d) -> p h d", d=64)
                if full:
                    k.op("dve", lambda e: e.tensor_tensor(out=tmp1.ap()[:, 0:W], in0=s2, in1=s2, op=ALU.mult), [src], [tmp1])
                    k.op("dve", lambda e: e.tensor_reduce(out=ms.ap()[:, 0:H], in_=tmp1.ap()[:, 0:W].rearrange("p (h d) -> p h d", d=64),
                                                          axis=AX.X, op=ALU.add), [tmp1], [ms])
                    k.op("act", lambda e: e.activation(out=ms.ap()[:, 0:H], in_=ms.ap()[:, 0:H], func=AF.Sqrt,
                                                       bias=self.eps_rms.ap(), scale=1.0 / 64.0), [ms, self.eps_rms], [ms])
                    k.op("dve", lambda e: e.reciprocal(out=ms.ap()[:, 0:H], in_=ms.ap()[:, 0:H]), [ms], [ms])
                    k.op("dve", lambda e: e.tensor_tensor(out=s3, in0=s3, in1=ms.ap()[:, 0:H].unsqueeze(2).to_broadcast([128, H, 64]),
                                                          op=ALU.mult), [src, ms], [src])
                    k.op("pool", lambda e: e.tensor_tensor(out=s3, in0=s3, in1=gn.ap().unsqueeze(1).to_broadcast([128, H, 64]),
                                                           op=ALU.mult), [src, gn], [src])
                d2 = dst.ap()[:, 0:W]
                if t < NTL:
                    v5 = s2.rearrange("p (h a b f) -> p h a b f", a=2, b=2, f=16)
                    x1 = v5[:, :, :, 0, :]
                    x2 = v5[:, :, :, 1, :]
                    o5 = d2.rearrange("p (h a b f) -> p h a b f", a=2, b=2, f=16)
                    cs = COS.ap()[:, t, :].rearrange("p (a f) -> p a f", a=2).unsqueeze(1).to_broadcast([128, H, 2, 16])
                    sn = SIN.ap()[:, t, :].rearrange("p (a f) -> p a f", a=2).unsqueeze(1).to_broadcast([128, H, 2, 16])
                    WH = W // 2
                    ta = tmp1.ap()[:, 0:WH].rearrange("p (h a f) -> p h a f", a=2, f=16)
                    tb = tmp2.ap()[:, 0:WH].rearrange("p (h a f) -> p h a f", a=2, f=16)
                    tc_ = tmp1.ap()[:, WH:W].rearrange("p (h a f) -> p h a f", a=2, f=16)
                    td = tmp2.ap()[:, WH:W].rearrange("p (h a f) -> p h a f", a=2, f=16)
                    k.op("dve", lambda e: e.tensor_tensor(out=ta, in0=x1, in1=cs, op=ALU.mult), [src, COS], [tmp1])
                    k.op("pool", lambda e: e.tensor_tensor(out=tb, in0=x2, in1=sn, op=ALU.mult), [src, SIN], [tmp2])
                    k.op("dve", lambda e: e.tensor_tensor(out=tc_, in0=x2, in1=cs, op=ALU.mult), [src, COS], [tmp1])
                    k.op("pool", lambda e: e.tensor_tensor(out=td, in0=x1, in1=sn, op=ALU.mult), [src, SIN], [tmp2])
                    k.op("dve", lambda e: e.tensor_tensor(out=o5[:, :, :, 0, :], in0=ta, in1=tb, op=ALU.subtract), [tmp1, tmp2], [dst])
                    k.op("dve", lambda e: e.tensor_tensor(out=o5[:, :, :, 1, :], in0=tc_, in1=td, op=ALU.add), [tmp1, tmp2], [dst])
                else:
                    k.op("dve", lambda e: e.tensor_copy(out=d2, in_=s2), [src], [dst])

            with k.scope():
                Wkv = k.sb("Wkv", [128, 8, 512], BF16)
                for kc in range(8):
                    k.dma("pool", Wkv.ap()[:, kc, :], wqkv_d[kc * 128:(kc + 1) * 128, 1024:1536], [], [Wkv])
                for t in range(NT):
                    xt, hT = front(t)
                    for kc in range(8):
                        k.op("pe", lambda e: e.matmul(PA.ap(), hT.ap()[:, kc * 128:(kc + 1) * 128], Wkv.ap()[:, kc, :],
                                                      start=(kc == 0), stop=(kc == 7)), [hT, Wkv], [PA], inc=(kc == 7))
                    if full:
                        k.op("act", lambda e: e.copy(out=qs.ap()[:, 0:512], in_=PA.ap()), [PA], [qs])
                    else:
                        k.op("dve", lambda e: e.tensor_tensor(out=qs.ap()[:, 0:512], in0=PA.ap(), in1=bias.ap()[:, 1024:1536], op=ALU.add),
                             [PA, bias], [qs])
                    k.op("act", lambda e: e.copy(out=V.ap()[:, t, :, 0:64], in_=qs.ap()[:, 256:512].rearrange("p (g d) -> p g d", d=64)),
                         [qs], [V])
                    process_qk(qs, 4, t, kn if full else None, qb)
                    for g in range(4):
                        k.op("pe", lambda e: e.transpose(PT1.ap()[0:64, g * 128:(g + 1) * 128], qb.ap()[:, g * 64:(g + 1) * 64], self.identb.ap()),
                             [qb, self.identb], [PT1], inc=(g == 3))
                    k.op("act", lambda e: e.copy(out=KT.ap()[:, :, t * 128:(t + 1) * 128],
                                                 in_=PT1.ap()[0:64, 0:512].rearrange("p (g n) -> p g n", g=4)), [PT1], [KT])

            with k.scope():
                Wq = k.sb("Wq", [128, 8, 1024], BF16)
                for kc in range(8):
                    k.dma("pool", Wq.ap()[:, kc, :], wqkv_d[kc * 128:(kc + 1) * 128, 0:1024], [], [Wq])
                Wo = k.sb("Wo", [64, 16, 1024], BF16)
                for hh in range(16):
                    k.dma("pool", Wo.ap()[:, hh, :], wo_d[hh * 64:(hh + 1) * 64, :], [], [Wo])
                modG = k.sb("modG", [128, D], F32)
                lng = k.sb("lng", [128, D], F32)
                lnb = k.sb("lnb", [128, D], F32)
                self.load_bc(lng, self.ln1_g[i - self.first_layer])
                self.load_bc(lnb, self.ln1_b[i - self.first_layer])
                PS = Rot([k.ps(f"PS{n}", [128, 512], F32) for n in range(2)])
                PO = k.ps("PO", [128, 512], F32)
                PBC = k.ps("PBC", [128, 512], F32)
                QT = k.sb("QT", [64, 2048], BF16)
                onT = k.sb("onT", [64, 2048], BF16)
                pTs = Rot([k.sb(f"pT{n}", [128, 512], BF16) for n in range(3)])
                lnd = k.sb("lnd", [128, 512], F32)
                rd = k.sb("rd", [128, 512], F32)
                osb = k.sb("osb", [64, 512], F32)
                xos = Rot([k.sb(f"xo{n}", [128, D], F32) for n in range(2)])
                st6 = k.sb("st6", [128, 12], F32)
                mvv = k.sb("mvv", [128, 2], F32)
                rstd = k.sb("rstd", [128, 1], F32)
                if not full:
                    mask = k.sb("mask", [128, 2, 128], BF16)
                    k.dma("pool", mask.ap(), self.c_mask, [], [mask])
                    e64 = k.sb("e64", [1, 65], BF16)
                    k.op("dve", lambda e: e.memset(e64.ap(), 0.0), [], [e64])
                    k.op("dve", lambda e: e.memset(e64.ap()[:, 64:65], 1.0), [e64], [e64])
                    sk = k.sb("sk", [1, 16], F32)
                    k.dma("sp", sk.ap(), self.win_sink[j:j + 1, :], [], [sk])
                    k.op("act", lambda e: e.activation(out=sk.ap(), in_=sk.ap(), func=AF.Exp), [sk], [sk])
                    esink = k.sb("esink", [1, 16, 128], BF16)
                    k.op("dve", lambda e: e.tensor_copy(out=esink.ap(), in_=sk.ap().unsqueeze(2).to_broadcast([1, 16, 128])), [sk], [esink])
                g_ty = [-1]
                for t in range(nq):
                    ty = 0 if t < NTL else 1
                    xt, hT = front(t)
                    if g_ty[0] != ty:
                        self.load_mod(i, ty, 2, modG)
                        g_ty[0] = ty
                    for n, P in enumerate((PA, PB)):
                        for kc in range(8):
                            k.op("pe", lambda e: e.matmul(P.ap(), hT.ap()[:, kc * 128:(kc + 1) * 128], Wq.ap()[:, kc, n * 512:(n + 1) * 512],
                                                          start=(kc == 0), stop=(kc == 7)), [hT, Wq], [P], inc=(kc == 7))
                        if full:
                            k.op("act", lambda e: e.copy(out=qs.ap()[:, n * 512:(n + 1) * 512], in_=P.ap()), [P], [qs])
                        else:
                            k.op("dve", lambda e: e.tensor_tensor(out=qs.ap()[:, n * 512:(n + 1) * 512], in0=P.ap(),
                                                                  in1=bias.ap()[:, n * 512:(n + 1) * 512], op=ALU.add), [P, bias], [qs])
                    process_qk(qs, 16, t, qn if full else None, qb)
                    for hh in range(16):
                        PT = PT0 if hh < 8 else PT1
                        k.op("pe", lambda e: e.transpose(PT.ap()[0:64, (hh % 8) * 128:(hh % 8 + 1) * 128], qb.ap()[:, hh * 64:(hh + 1) * 64],
                                                         self.identb.ap()), [qb, self.identb], [PT], inc=(hh % 8 == 7))
                    k.op("act", lambda e: e.copy(out=QT.ap()[:, 0:1024], in_=PT0.ap()[0:64, :]), [PT0], [QT])
                    k.op("act", lambda e: e.copy(out=QT.ap()[:, 1024:2048], in_=PT1.ap()[0:64, :]), [PT1], [QT])
                    if t >= NTL:
                        chunks = [(NTL, None), (NTL + 1, None)]
                    elif full:
                        chunks = [(c, None) for c in range(NT)]
                    else:
                        chunks = []
                        if t > 0:
                            chunks.append((t - 1, 0))
                        chunks.append((t, None))
                        if t < NTL - 1:
                            chunks.append((t + 1, 1))
                        chunks += [(NTL, None), (NTL + 1, None)]
                    for g in range(4):
                        for ci, (jc, m) in enumerate(chunks):
                            S = PS.next()
                            k.op("pe", lambda e: e.matmul(S.ap(), KT.ap()[:, g, jc * 128:(jc + 1) * 128], QT.ap()[:, g * 512:(g + 1) * 512],
                                                          start=True, stop=True), [KT, QT], [S])
                            pT = pTs.next()
                            k.op("act", lambda e: e.activation(out=pT.ap(), in_=S.ap(), func=AF.Exp, scale=0.125), [S], [pT])
                            if m is not None:
                                p3 = pT.ap().rearrange("p (h n) -> p h n", h=4)
                                k.op("dve", lambda e: e.tensor_tensor(out=p3, in0=p3, in1=mask.ap()[:, m, :].unsqueeze(1).to_broadcast([128, 4, 128]),
                                                                      op=ALU.mult), [pT, mask], [pT])
                            lastc = (ci == len(chunks) - 1) and full
                            k.op("pe", lambda e: e.matmul(PO.ap()[0:65, :], V.ap()[:, jc, g, :], pT.ap(), start=(ci == 0), stop=lastc),
                                 [V, pT], [PO])
                        if not full:
                            k.op("pe", lambda e: e.matmul(PO.ap()[0:65, :], e64.ap(), esink.ap()[:, 4 * g:4 * g + 4, :].rearrange("p h n -> p (h n)"),
                                                          start=False, stop=True), [e64, esink], [PO])
                        k.op("act", lambda e: e.activation(out=lnd.ap()[64:65, :], in_=PO.ap()[64:65, :], func=AF.Ln), [PO], [lnd])
                        k.op("act", lambda e: e.activation(out=rd.ap()[64:65, :], in_=lnd.ap()[64:65, :], func=AF.Exp, scale=-1.0), [lnd], [rd])
                        k.op("act", lambda e: e.copy(out=osb.ap(), in_=PO.ap()[0:64, :]), [PO], [osb])
                        k.op("pe", lambda e: e.matmul(PBC.ap()[0:64, :], self.onesf.ap()[64:65, :], rd.ap()[64:65, :], start=True, stop=True),
                             [self.onesf, rd], [PBC])
                        k.op("dve", lambda e: e.tensor_tensor(out=onT.ap()[:, g * 512:(g + 1) * 512], in0=osb.ap(), in1=PBC.ap()[0:64, :],
                                                              op=ALU.mult), [osb, PBC], [onT])
                    for n, P in enumerate((PA, PB)):
                        for hh in range(16):
                            k.op("pe", lambda e: e.matmul(P.ap(), onT.ap()[:, hh * 128:(hh + 1) * 128], Wo.ap()[:, hh, n * 512:(n + 1) * 512],
                                                          start=(hh == 0), stop=(hh == 15)), [onT, Wo], [P], inc=(hh == 15))
                    xo = xos.next()
                    self.resid_ln([(PA.ap(), PA), (PB.ap(), PB)], xt, modG, lng, lnb, tmp2, xo, st6, mvv, rstd)
                    self.store_x(t, xo, last)

    def store_x(self, t, xo, last):
        k = self.k
        if last and (t < NTL or not self.final):
            k.dma("sp", self.out[t * 128:(t + 1) * 128, :], xo.ap(), [xo], [k.out_buf])
        else:
            k.dma("sp", self.X[t * 128:(t + 1) * 128, :], xo.ap(), [xo], [self.Xd[t]])

    def conv_layer(self, i, j, need_ctx, last):
        k = self.k
        nq = NT if need_ctx else NTL
        NTOK = NT * 128
        j = self.jc[j]
        with k.scope():
            PT0 = k.ps("cPT0", [128, 1024], BF16)
            modA = k.sb("cmodA", [128, D], F32)
            modB = k.sb("cmodB", [128, D], F32)
            with k.scope():
                hTall = k.sb("hTall", [128, 8, NTOK], BF16)
                xts = Rot([k.sb(f"cxt{n}", [128, D], F32) for n in range(2)])
                tmp1 = k.sb("ctmp1", [128, D], F32)
                hbs = Rot([k.sb(f"chb{n}", [128, D], BF16) for n in range(2)])
                cur_ty = -1
                for t in range(NT):
                    ty = 0 if t < NTL else 1
                    if cur_ty != ty:
                        self.load_mod(i, ty, 1, modA)
                        self.load_mod(i, ty, 0, modB)
                        cur_ty = ty
                    xt = xts.next()
                    k.dma("sp", xt.ap(), self.X[t * 128:(t + 1) * 128, :], [self.Xd[t]], [xt])
                    k.op("pool", lambda e: e.tensor_tensor(out=tmp1.ap(), in0=xt.ap(), in1=modA.ap(), op=ALU.mult), [xt, modA], [tmp1])
                    hb = hbs.next()
                    k.op("dve", lambda e: e.tensor_tensor(out=hb.ap(), in0=tmp1.ap(), in1=modB.ap(), op=ALU.add), [tmp1, modB], [hb])
                    for c in range(8):
                        k.op("pe", lambda e: e.transpose(PT0.ap()[:, c * 128:(c + 1) * 128], hb.ap()[:, c * 128:(c + 1) * 128], self.identb.ap()),
                             [hb, self.identb], [PT0], inc=(c == 7))
                    k.op("act", lambda e: e.copy(out=hTall.ap()[:, :, t * 128:(t + 1) * 128],
                                                 in_=PT0.ap().rearrange("p (c n) -> p c n", c=8)), [PT0], [hTall])
                cw = k.sb("cw", [128, 8, 3], F32)
                k.dma("sp", cw.ap(), self.conv_wT[j], [], [cw])
                bg = k.sb("cbg", [128, NTOK], F32)
                cx = k.sb("ccx", [128, NTOK + 4], F32)
                yv = k.sb("cyv", [128, NTOK], F32)
                k.op("dve", lambda e: e.memset(cx.ap()[:, 0:1], 0.0), [], [cx])
                k.op("dve", lambda e: e.memset(cx.ap()[:, L + 1:L + 3], 0.0), [cx], [cx])
                k.op("dve", lambda e: e.memset(cx.ap()[:, NTOK + 3:NTOK + 4], 0.0), [cx], [cx])
                mTs = Rot([k.sb(f"cmT{n}", [128, NTOK], BF16) for n in range(1)])
                Wcs = Rot([k.sb(f"cWc{n}", [128, 8, 3, 128], BF16) for n in range(2)])
                csb = k.sb("ccsb", [128, 512], F32)
                PBk = k.ps("cPB", [128, 512], F32)
                PCk = k.ps("cPC", [128, 512], F32)
                PXk = k.ps("cPX", [128, 512], F32)
                groups = [(g * 512, 512) for g in range(8)] + [(L, 256)]
                for jf in range(8):
                    Wc = Wcs.next()
                    for kc in range(8):
                        k.dma("pool", Wc.ap()[:, kc, :, :],
                              self.conv_win[j][kc * 128:(kc + 1) * 128, :].rearrange("p (s n) -> p s n", s=3)[:, :, jf * 128:(jf + 1) * 128],
                              [], [Wc])
                    for (t0, n) in groups:
                        off = 1 + t0 if t0 < L else 3 + t0
                        for s, P in enumerate((PBk, PCk, PXk)):
                            for kc in range(8):
                                k.op("pe", lambda e: e.matmul(P.ap()[:, 0:n], Wc.ap()[:, kc, s, :], hTall.ap()[:, kc, t0:t0 + n],
                                                              start=(kc == 0), stop=(kc == 7)), [Wc, hTall], [P], inc=(kc == 7))
                        k.op("act", lambda e: e.copy(out=bg.ap()[:, t0:t0 + n], in_=PBk.ap()[:, 0:n]), [PBk], [bg])
                        k.op("act", lambda e: e.copy(out=csb.ap()[:, 0:n], in_=PCk.ap()[:, 0:n]), [PCk], [csb])
                        k.op("dve", lambda e: e.tensor_tensor(out=cx.ap()[:, off:off + n], in0=csb.ap()[:, 0:n], in1=PXk.ap()[:, 0:n], op=ALU.mult),
                             [csb, PXk], [cx])
                    mT = mTs.next()
                    for (t0, n) in [(c * 1024, 1024) for c in range(4)] + [(L, 256)]:
                        off = 1 + t0 if t0 < L else 3 + t0
                        k.op("dve", lambda e: e.tensor_scalar(out=yv.ap()[:, t0:t0 + n], in0=cx.ap()[:, off - 1:off - 1 + n],
                                                              scalar1=cw.ap()[:, jf, 0:1], scalar2=None, op0=ALU.mult), [cx, cw], [yv])
                        for s in (1, 2):
                            k.op("dve", lambda e: e.scalar_tensor_tensor(out=yv.ap()[:, t0:t0 + n], in0=cx.ap()[:, off - 1 + s:off - 1 + s + n],
                                                                         scalar=cw.ap()[:, jf, s:s + 1], in1=yv.ap()[:, t0:t0 + n],
                                                                         op0=ALU.mult, op1=ALU.add), [cx, cw, yv], [yv])
                        k.op("pool", lambda e: e.tensor_tensor(out=mT.ap()[:, t0:t0 + n], in0=bg.ap()[:, t0:t0 + n], in1=yv.ap()[:, t0:t0 + n],
                                                               op=ALU.mult), [bg, yv], [mT])
                    k.dma("sp", self.MT[jf], mT.ap(), [mT], [self.MTd])
            with k.scope():
                Wout = k.sb("cWout", [128, 8, D], BF16)
                for kc in range(8):
                    k.dma("pool", Wout.ap()[:, kc, :], self.conv_wout[j][kc * 128:(kc + 1) * 128, :], [], [Wout])
                modG = k.sb("cmodG", [128, D], F32)
                lng = k.sb("clng", [128, D], F32)
                lnb = k.sb("clnb", [128, D], F32)
                self.load_bc(lng, self.ln1_g[i - self.first_layer])
                self.load_bc(lnb, self.ln1_b[i - self.first_layer])
                PA = k.ps("cPA", [128, 512], F32)
                PB = k.ps("cPB2", [128, 512], F32)
                xts = Rot([k.sb(f"dxt{n}", [128, D], F32) for n in range(2)])
                mts = Rot([k.sb(f"dmt{n}", [128, 8, 128], BF16) for n in range(2)])
                xos = Rot([k.sb(f"dxo{n}", [128, D], F32) for n in range(2)])
                u = k.sb("du", [128, D], F32)
                st6 = k.sb("dst6", [128, 12], F32)
                mvv = k.sb("dmvv", [128, 2], F32)
                rstd = k.sb("drstd", [128, 1], F32)
                g_ty = -1
                for t in range(nq):
                    ty = 0 if t < NTL else 1
                    if g_ty != ty:
                        self.load_mod(i, ty, 2, modG)
                        g_ty = ty
                    xt = xts.next()
                    k.dma("sp", xt.ap(), self.X[t * 128:(t + 1) * 128, :], [self.Xd[t]], [xt])
                    mt = mts.next()
                    k.dma("act", mt.ap(), self.MT[:, :, t * 128:(t + 1) * 128].rearrange("j p n -> p j n"), [self.MTd], [mt])
                    for n, P in enumerate((PA, PB)):
                        for jf in range(8):
                            k.op("pe", lambda e: e.matmul(P.ap(), mt.ap()[:, jf, :], Wout.ap()[:, jf, n * 512:(n + 1) * 512],
                                                          start=(jf == 0), stop=(jf == 7)), [mt, Wout], [P], inc=(jf == 7))
                    xo = xos.next()
                    self.resid_ln([(PA.ap(), PA), (PB.ap(), PB)], xt, modG, lng, lnb, u, xo, st6, mvv, rstd)
                    self.store_x(t, xo, last)

    def moe_layer(self, i, need_ctx, last):
        k = self.k
        nt = NT if need_ctx else NTL
        if nt == NT:
            sizes = [7, 7, 7, 7, 6]
        else:
            sizes = [7, 7, 6, 6, 6]
        MAXT = max(sizes)
        groups = []
        a = 0
        for s in sizes:
            groups.append(list(range(a, a + s)))
            a += s
        with k.scope():
            rw = k.sb("rw", [128, 8, NE], F32)
            k.dma("sp", rw.ap(), self.router_w[i - self.first_layer].rearrange("(kc p) e -> p kc e", p=128), [], [rw])
            rb = k.sb("rb", [128, NE], F32)
            self.load_bc(rb, self.router_b[i - self.first_layer])
            bgu = k.sb("bgu", [128, NE, 16], F32)
            k.dma("act", bgu.ap(), self.expert_bguT[i - self.first_layer], [], [bgu])
            Bd = k.sb("Bd", [NE, D], F32)
            k.dma("act", Bd.ap(), self.expert_bdown[i - self.first_layer], [], [Bd])
            hT2 = k.sb("hT2", [128, 8, MAXT * 128], BF16)
            yacc = k.sb("yacc", [128, MAXT, D], F32)
            GTall = k.sb("GTall", [NE, MAXT * 128], F32)
            actT = k.sb("actT", [128, 8, MAXT * 128], BF16)
            gbcs = Rot([k.sb(f"gbc{n}", [128, MAXT * 128], F32) for n in range(1)])
            Wgs = Rot([k.sb(f"Wg{n}", [128, 8, 2 * D], BF16) for n in range(2)])
            Wds = Rot([k.sb(f"Wd{n}", [128, 8, D], BF16) for n in range(1)])
            modA = k.sb("mmodA", [128, D], F32)
            modB = k.sb("mmodB", [128, D], F32)
            modG = modA
            lng = modB
            lnb = k.sb("mlnb", [128, D], F32)
            self.load_bc(lnb, self.ln2_b[i - self.first_layer])
            xts = Rot([k.sb(f"mxt{n}", [128, D], F32) for n in range(2)])
            tmp1 = k.sb("mtmp1", [128, D], F32)
            hTf = k.sb("mhTf", [128, D], F32)
            xos = Rot([k.sb(f"mxo{n}", [128, D], F32) for n in range(1)])
            st6 = k.sb("mst6", [128, 12], F32)
            mvv = k.sb("mmvv", [128, 2], F32)
            rstd = k.sb("mrstd", [128, 1], F32)
            lg = k.sb("lg", [128, NE], F32)
            top8 = k.sb("top8", [128, 8], F32)
            msk = k.sb("msk", [128, NE], F32)
            nmx = k.sb("nmx", [128, 1], F32)
            ssum = k.sb("ssum", [128, 1], F32)
            Gt = k.sb("Gt", [128, 128], F32)
            k.op("dve", lambda e: e.memset(Gt.ap(), 0.0), [], [Gt])
            GTs = Rot([k.sb(f"GTs{n}", [NE, 128], F32) for n in range(2)])
            g1s = Rot([k.sb(f"g1_{n}", [128, 512], F32) for n in range(2)])
            sgs = Rot([k.sb(f"sg_{n}", [128, 512], F32) for n in range(2)])
            u1s = Rot([k.sb(f"u1_{n}", [128, 512], F32) for n in range(2)])
            t1s = Rot([k.sb(f"t1_{n}", [128, 512], F32) for n in range(2)])
            P0 = k.ps("mP0", [128, 512], F32)
            P1 = k.ps("mP1", [128, 512], F32)
            PGs = Rot([k.ps(f"mPG{n}", [128, 512], F32) for n in range(2)])
            PUs = Rot([k.ps(f"mPU{n}", [128, 512], F32) for n in range(2)])
            PYs = Rot([k.ps(f"mPY{n}", [128, 512], F32) for n in range(2)])
            cur = {"a": -1, "g": -1}
            import os
            STOP = int(os.environ.get("MOE_STOP", 99))
            for grp in groups[:int(os.environ.get('MOE_NGRP', 99))]:
                ntg = len(grp)
                ntok = ntg * 128
                tok0 = grp[0] * 128
                for a, t in enumerate(grp):
                    ty = 0 if t < NTL else 1
                    if cur["a"] != ty:
                        self.load_mod(i, ty, 4, modA)
                        self.load_mod(i, ty, 3, modB)
                        cur["a"] = ty
                    xt = xts.next()
                    k.dma("sp", xt.ap(), self.X[t * 128:(t + 1) * 128, :], [self.Xd[t]], [xt])
                    k.op("dve", lambda e: e.tensor_tensor(out=tmp1.ap(), in0=xt.ap(), in1=modA.ap(), op=ALU.mult), [xt, modA], [tmp1])
                    k.op("dve", lambda e: e.tensor_tensor(out=tmp1.ap(), in0=tmp1.ap(), in1=modB.ap(), op=ALU.add), [tmp1, modB], [tmp1])
                    if STOP <= 1:
                        continue
                    for c in range(8):
                        P = P0 if c < 4 else P1
                        k.op("pe", lambda e: e.transpose(P.ap()[:, (c % 4) * 128:(c % 4 + 1) * 128], tmp1.ap()[:, c * 128:(c + 1) * 128], self.identf.ap()),
                             [tmp1, self.identf], [P], inc=(c % 4 == 3))
                    if STOP <= 2:
                        continue
                    for hh, P in enumerate((P0, P1)):
                        S3 = os.environ.get("MOE_S3", "ab")
                        if "a" in S3:
                            k.op("dve", lambda e: e.tensor_copy(out=hTf.ap()[:, hh * 512:(hh + 1) * 512], in_=P.ap()), [P], [hTf])
                        if "b" in S3:
                            k.op("dve", lambda e: e.tensor_copy(out=hT2.ap()[:, hh * 4:(hh + 1) * 4, a * 128:(a + 1) * 128],
                                                                in_=P.ap().rearrange("p (c n) -> p c n", c=4)), [P], [hT2])
                        if "c" in S3:
                            for c4 in range(4):
                                k.op("dve", lambda e: e.tensor_copy(out=hT2.ap()[:, hh * 4 + c4, a * 128:(a + 1) * 128],
                                                                    in_=P.ap()[:, c4 * 128:(c4 + 1) * 128]), [P], [hT2])
                    if STOP <= 3:
                        continue
                    PR = PGs.next()
                    for kc in range(8):
                        k.op("pe", lambda e: e.matmul(PR.ap()[:, 0:NE], hTf.ap()[:, kc * 128:(kc + 1) * 128], rw.ap()[:, kc, :],
                                                      start=(kc == 0), stop=(kc == 7)), [hTf, rw], [PR], inc=(kc == 7))
                    if STOP <= 4:
                        continue
                    k.op("dve", lambda e: e.tensor_tensor(out=lg.ap(), in0=PR.ap()[:, 0:NE], in1=rb.ap(), op=ALU.add), [PR, rb], [lg])
                    k.op("dve", lambda e: e.max(out=top8.ap(), in_=lg.ap()), [lg], [top8])
                    k.op("dve", lambda e: e.tensor_scalar(out=msk.ap(), in0=lg.ap(), scalar1=top8.ap()[:, 3:4], scalar2=None, op0=ALU.is_ge),
                         [lg, top8], [msk])
                    k.op("dve", lambda e: e.tensor_scalar(out=nmx.ap(), in0=top8.ap()[:, 0:1], scalar1=-1.0, scalar2=None, op0=ALU.mult),
                         [top8], [nmx])
                    k.op("act", lambda e: e.activation(out=lg.ap(), in_=lg.ap(), func=AF.Exp, bias=nmx.ap(), scale=1.0), [lg, nmx], [lg])
                    k.op("dve", lambda e: e.tensor_tensor(out=lg.ap(), in0=lg.ap(), in1=msk.ap(), op=ALU.mult), [lg, msk], [lg])
                    k.op("dve", lambda e: e.tensor_reduce(out=ssum.ap(), in_=lg.ap(), axis=AX.X, op=ALU.add), [lg], [ssum])
                    k.op("dve", lambda e: e.reciprocal(out=ssum.ap(), in_=ssum.ap()), [ssum], [ssum])
                    k.op("dve", lambda e: e.tensor_scalar(out=Gt.ap()[:, 0:NE], in0=lg.ap(), scalar1=ssum.ap(), scalar2=None, op0=ALU.mult),
                         [lg, ssum], [Gt])
                    if STOP <= 5:
                        continue
                    PG_ = PUs.next()
                    k.op("pe", lambda e: e.transpose(PG_.ap()[:, 0:128], Gt.ap(), self.identf.ap()), [Gt, self.identf], [PG_])
                    GT_ = GTs.next()
                    k.op("dve", lambda e: e.tensor_copy(out=GT_.ap(), in_=PG_.ap()[0:NE, 0:128]), [PG_], [GT_])
                    k.op("dve", lambda e: e.tensor_copy(out=GTall.ap()[:, a * 128:(a + 1) * 128], in_=GT_.ap()), [GT_], [GTall])
                    k.dma("sp", self.GTD[:, t * 128:(t + 1) * 128], GT_.ap(), [GT_], [self.GTDd])
                    if STOP <= 6:
                        continue
                    for n in range(2):
                        PY = PYs.next()
                        k.op("pe", lambda e: e.matmul(PY.ap(), GTall.ap()[:, a * 128:(a + 1) * 128], Bd.ap()[:, n * 512:(n + 1) * 512],
                                                      start=True, stop=True), [GTall, Bd], [PY])
                        k.op("act", lambda e: e.copy(out=yacc.ap()[:, a, n * 512:(n + 1) * 512], in_=PY.ap()), [PY], [yacc])
                if STOP < 99:
                    continue
                tgs = []
                o = 0
                while o < ntok:
                    n = min(512, ntok - o)
                    tgs.append((o, n))
                    o += n
                import os
                for ex in range(int(os.environ.get('MOE_NEXP', NE))):
                    nexp = int(os.environ.get('MOE_NEXP', NE))
                    Wd = Wds.next()
                    if ex == 0:
                        Wg = Wgs.next()
                        for kc in range(8):
                            k.dma("pool", Wg.ap()[:, kc, :], self.expert_wgu[i - self.first_layer, 0, kc * 128:(kc + 1) * 128, :], [], [Wg])
                        for kc in range(8):
                            k.dma("pool", Wd.ap()[:, kc, :], self.expert_wdown[i - self.first_layer, 0, kc * 128:(kc + 1) * 128, :], [], [Wd])
                    else:
                        Wg = Wg_next
                    if ex + 1 < nexp:
                        Wg_next = Wgs.next()
                        for kc in range(8):
                            k.dma("pool", Wg_next.ap()[:, kc, :], self.expert_wgu[i - self.first_layer, ex + 1, kc * 128:(kc + 1) * 128, :], [], [Wg_next])
                    gbc = gbcs.next()
                    k.dma("sp", gbc.ap()[:, 0:ntok], self.GTD[ex, tok0:tok0 + ntok].partition_broadcast(128), [self.GTDd], [gbc])
                    for f in range(8):
                        for (o, n) in tgs:
                            PG = PGs.next()
                            PU = PUs.next()
                            for (P, cb) in ((PG, f * 128), (PU, D + f * 128)):
                                for kc in range(8):
                                    k.op("pe", lambda e: e.matmul(P.ap()[:, 0:n], Wg.ap()[:, kc, cb:cb + 128], hT2.ap()[:, kc, o:o + n],
                                                                  start=(kc == 0), stop=(kc == 7)), [Wg, hT2], [P], inc=(kc == 7))
                            g1 = g1s.next()
                            sg = sgs.next()
                            u1 = u1s.next()
                            t1 = t1s.next()
                            k.op("dve", lambda e: e.tensor_scalar(out=g1.ap()[:, 0:n], in0=PG.ap()[:, 0:n], scalar1=bgu.ap()[:, ex, f:f + 1],
                                                                  scalar2=7.0, op0=ALU.add, op1=ALU.min), [PG, bgu], [g1])
                            k.op("act", lambda e: e.activation(out=sg.ap()[:, 0:n], in_=g1.ap()[:, 0:n], func=AF.Silu, scale=1.702), [g1], [sg])
                            k.op("dve", lambda e: e.tensor_scalar(out=u1.ap()[:, 0:n], in0=PU.ap()[:, 0:n], scalar1=bgu.ap()[:, ex, 8 + f:9 + f],
                                                                  scalar2=7.0, op0=ALU.add, op1=ALU.min), [PU, bgu], [u1])
                            k.op("dve", lambda e: e.tensor_scalar(out=u1.ap()[:, 0:n], in0=u1.ap()[:, 0:n], scalar1=-7.0, scalar2=1.0,
                                                                  op0=ALU.max, op1=ALU.add), [u1], [u1])
                            k.op("dve", lambda e: e.scalar_tensor_tensor(out=t1.ap()[:, 0:n], in0=sg.ap()[:, 0:n], scalar=1.0 / 1.702,
                                                                         in1=u1.ap()[:, 0:n], op0=ALU.mult, op1=ALU.mult), [sg, u1], [t1])
                            k.op("dve", lambda e: e.tensor_tensor(out=actT.ap()[:, f, o:o + n], in0=t1.ap()[:, 0:n], in1=gbc.ap()[:, o:o + n],
                                                                  op=ALU.mult), [t1, gbc], [actT])
                    for a in range(ntg):
                        for n in range(2):
                            PY = PYs.next()
                            for f in range(8):
                                k.op("pe", lambda e: e.matmul(PY.ap(), actT.ap()[:, f, a * 128:(a + 1) * 128], Wd.ap()[:, f, n * 512:(n + 1) * 512],
                                                              start=(f == 0), stop=(f == 7)), [actT, Wd], [PY], inc=(f == 7))
                            ysl = yacc.ap()[:, a, n * 512:(n + 1) * 512]
                            k.op("dve", lambda e: e.tensor_tensor(out=ysl, in0=PY.ap(), in1=ysl, op=ALU.add), [PY, yacc], [yacc])
                    if ex + 1 < nexp:
                        for kc in range(8):
                            k.dma("pool", Wd.ap()[:, kc, :], self.expert_wdown[i - self.first_layer, ex + 1, kc * 128:(kc + 1) * 128, :], [], [Wd])
                cur["a"] = -1
                cur["g"] = -1
                self.load_bc(lng, self.ln2_g[i - self.first_layer])
                for a, t in enumerate(grp):
                    ty = 0 if t < NTL else 1
                    if cur["g"] != ty:
                        self.load_mod(i, ty, 5, modG)
                        cur["g"] = ty
                    xt = xts.next()
                    k.dma("sp", xt.ap(), self.X[t * 128:(t + 1) * 128, :], [self.Xd[t]], [xt])
                    xo = xos.next()
                    self.resid_ln([(yacc.ap()[:, a, 0:512], yacc), (yacc.ap()[:, a, 512:1024], yacc)], xt, modG, lng, lnb, tmp1, xo, st6, mvv, rstd)
                    self.store_x(t, xo, last)

    def build(self):
        k = self.k
        self.consts()
        self.prologue()
        for i in range(self.first_layer, self.first_layer + self.n_layers):
            kind, j = i % 3, i // 3
            need_ctx = i < DEPTH - 1
            last = (i == self.first_layer + self.n_layers - 1)
            if self.debug != "moe_only":
                if kind == 1:
                    self.conv_layer(i, j, need_ctx, last and self.debug == "mixer_only")
                else:
                    self.attention_layer(i, kind == 2, j, need_ctx, last and self.debug == "mixer_only")
            if self.debug != "mixer_only":
                self.moe_layer(i, need_ctx, last)
        k.finish()
        return self.nc


def _rope_tables():
    n_freq = 16
    inv = (10000.0 ** (-np.arange(n_freq, dtype=np.float32) / n_freq)).astype(np.float32)
    tok = np.arange(L)
    row = (tok // 64).astype(np.float32)
    col = (tok % 64).astype(np.float32)
    ang = np.stack([row[:, None] * inv, col[:, None] * inv], axis=1).astype(np.float32)
    cos = np.cos(ang).astype(np.float32).reshape(NTL, 128, 32).transpose(1, 0, 2)
    sin = np.sin(ang).astype(np.float32).reshape(NTL, 128, 32).transpose(1, 0, 2)
    return np.ascontiguousarray(cos), np.ascontiguousarray(sin)


def make_in_maps(inputs, cores, first_layer=0, n_layers=DEPTH, xs=None):
    f = lambda a: np.ascontiguousarray(np.asarray(a, dtype=np.float32))
    layers = list(range(first_layer, first_layer + n_layers))
    cos, sin = _rope_tables()
    p = np.arange(128)
    mask = np.zeros((128, 2, 128), np.float32)
    mask[:, 0, :] = (p[:, None] >= p[None, :])
    mask[:, 1, :] = (p[:, None] <= p[None, :])
    shared = {"c_ident": np.eye(128, dtype=np.float32), "c_cos": cos, "c_sin": sin, "c_mask": mask}
    sl = slice(first_layer, first_layer + n_layers)
    for name in ("mod_w", "mod_b", "ln1_g", "ln1_b", "ln2_g", "ln2_b", "router_w", "router_b",
                 "expert_wgu", "expert_wdown", "expert_bdown"):
        shared[name] = f(inputs[name][sl])
    shared["expert_bguT"] = np.ascontiguousarray(f(inputs["expert_bgu"][sl]).reshape(n_layers, NE, 16, 128).transpose(0, 3, 1, 2))

    def pick(names, js, extra=None):
        js = sorted(js)
        for name in names:
            a = np.asarray(inputs[name])
            if js:
                shared[name] = f(a[js])
            else:
                shared[name] = np.zeros((1,) + a.shape[1:], np.float32)

    pick(("win_wqkv", "win_bqkv", "win_sink", "win_wo"), {i // 3 for i in layers if i % 3 == 0})
    jc = sorted({i // 3 for i in layers if i % 3 == 1})
    pick(("conv_win", "conv_wout"), jc)
    cw = f(inputs["conv_w"])
    if jc:
        shared["conv_wT"] = np.ascontiguousarray(cw[jc].reshape(len(jc), 3, 8, 128).transpose(0, 3, 2, 1))
    else:
        shared["conv_wT"] = np.zeros((1, 128, 8, 3), np.float32)
    pick(("full_wqkv", "full_qnorm", "full_knorm", "full_wo"), {i // 3 for i in layers if i % 3 == 2})
    x = f(inputs["x"])
    ctx = f(inputs["ctx"])
    c = f(inputs["c"])
    c_ctx = f(inputs["c_ctx"])
    maps = []
    for n, b in enumerate(cores):
        m = dict(shared)
        if xs is None:
            m["x"] = x[b]
            m["ctx"] = ctx[b]
        else:
            m["x"] = np.ascontiguousarray(xs[n][:L])
            m["ctx"] = np.ascontiguousarray(xs[n][L:])
        cc = np.stack([c[b].reshape(8, 128), c_ctx.reshape(8, 128)], axis=-1)
        m["ccT"] = np.ascontiguousarray(cc.transpose(1, 0, 2).reshape(128, 16))
        maps.append(m)
    return maps


LAUNCH_SPLIT = [(0, 4)]


def kernel(**inputs):
    cores = list(range(8))
    xs = None
    for li, (first, nl) in enumerate(LAUNCH_SPLIT):
        final = (li == len(LAUNCH_SPLIT) - 1)
        prog = Prog(n_layers=nl, first_layer=first, final=final)
        nc = prog.build()
        maps = make_in_maps(inputs, cores, first, nl, xs)
        res = run_bass_kernel_spmd(nc, maps, core_ids=cores)
        if final:
            return np.stack([np.asarray(r["out"], dtype=np.float32) for r in res.results], axis=0)
        xs = [np.asarray(r["xs"], dtype=np.float32) for r in res.results]
```
